# Optimizing a Trainium2 kernel written in Bass

```python
import jax, jax.numpy as jnp
from jax import lax

D_MODEL = 1024
BATCH = 8
SEQ = 4096
DEPTH = 1

FOX_HEADS = 8
FOX_HEAD_DIM = 64
FOX_WIDTH = FOX_HEADS * FOX_HEAD_DIM
HGRN_HEADS = 4
HGRN_EXPAND = 128
HGRN_WIDTH = D_MODEL - FOX_WIDTH
HGRN_HEAD_V = HGRN_WIDTH // HGRN_HEADS
HGRN_KEY_WIDTH = HGRN_HEADS * HGRN_EXPAND
MIX_WIDTH = FOX_WIDTH + HGRN_WIDTH
IN_SPLITS = (FOX_WIDTH, FOX_WIDTH, FOX_WIDTH, FOX_HEADS,
             HGRN_KEY_WIDTH, HGRN_KEY_WIDTH, HGRN_WIDTH, HGRN_WIDTH)
IN_WIDTH = sum(IN_SPLITS)
Q_BLOCK = 128
HGRN_CHUNK = 64
PEER_HEADS = 8
PEER_NKEYS = 128
PEER_NEXPERTS = PEER_NKEYS * PEER_NKEYS
PEER_QDIM = 256
PEER_HALF = PEER_QDIM // 2
PEER_TOPK = 16
PEER_TOKEN_BLOCK = 128
FOX_FGATE_BIAS_INIT = 2.0
LN_EPS = 1e-5
RMS_EPS = 1e-6
DEEPNORM_ALPHA = (2 * DEPTH) ** 0.25
DEEPNORM_BETA = (8 * DEPTH) ** -0.25

kernel_name = "hybrid_fox_hgrn2_peer_deepnorm"

F32 = jnp.float32


def _layer_norm(x, g, b):
    xf = x.astype(F32)
    mu = jnp.mean(xf, axis=-1, keepdims=True)
    var = jnp.mean(jnp.square(xf - mu), axis=-1, keepdims=True)
    y = (xf - mu) * lax.rsqrt(var + LN_EPS) * g.astype(F32) + b.astype(F32)
    return y.astype(x.dtype)


def _head_rms_norm(x, g, out_dtype):
    xf = x.astype(F32)
    y = xf * lax.rsqrt(jnp.mean(jnp.square(xf), axis=-1, keepdims=True) + RMS_EPS) * g.astype(F32)
    return y.astype(out_dtype)


def _split_cols(proj):
    outs, start = [], 0
    for w in IN_SPLITS:
        outs.append(proj[..., start:start + w])
        start += w
    return outs


def _fox_attention(q, k, v, log_f):
    B, S, H, Dh = q.shape
    nb = S // Q_BLOCK
    c = jnp.cumsum(log_f, axis=1)
    cT = c.transpose(0, 2, 1)
    kf = k.astype(F32)
    scale = Dh ** -0.5
    qb = q.astype(F32).reshape(B, nb, Q_BLOCK, H, Dh).transpose(1, 0, 2, 3, 4)
    cb = c.reshape(B, nb, Q_BLOCK, H).transpose(1, 0, 2, 3)
    key_pos = jnp.arange(S)

    def block(args):
        i, q_i, c_i = args
        logits = jnp.einsum('bqhd,bkhd->bhqk', q_i, kf) * scale
        logits = logits + c_i.transpose(0, 2, 1)[..., None] - cT[:, :, None, :]
        q_pos = i * Q_BLOCK + jnp.arange(Q_BLOCK)
        mask = key_pos[None, :] <= q_pos[:, None]
        logits = jnp.where(mask, logits, -jnp.inf)
        p = jax.nn.softmax(logits, axis=-1)
        return jnp.einsum('bhqk,bkhd->bqhd', p.astype(v.dtype), v)

    out = lax.map(block, (jnp.arange(nb), qb, cb))
    return out.transpose(1, 0, 2, 3, 4).reshape(B, S, H, Dh)


def _hgrn2_recurrence(q, k, v, log_f):
    B, S, H, DK = q.shape
    DV = v.shape[-1]
    C = HGRN_CHUNK
    n = S // C

    def to_chunks(t):
        return t.reshape(B, n, C, H, t.shape[-1]).transpose(1, 0, 3, 2, 4)

    qc, kc, vc, gc = to_chunks(q), to_chunks(k), to_chunks(v), to_chunks(log_f)
    causal = jnp.tril(jnp.ones((C, C), dtype=bool))

    def step(state, inp):
        q_i, k_i, v_i, g_i = inp
        b = jnp.cumsum(g_i, axis=2)
        o_inter = jnp.einsum('bhtk,bhkv->bhtv', q_i * jnp.exp(b), state)
        diff = b[:, :, :, None, :] - b[:, :, None, :, :]
        decay = jnp.exp(jnp.where(causal[:, :, None], diff, -jnp.inf))
        scores = jnp.einsum('bhtk,bhtsk,bhsk->bhts', q_i, decay, k_i)
        o = o_inter + jnp.einsum('bhts,bhsv->bhtv', scores, v_i)
        b_last = b[:, :, -1:, :]
        k_dec = k_i * jnp.exp(b_last - b)
        new_state = jnp.exp(b_last[:, :, 0, :])[..., None] * state + jnp.einsum('bhsk,bhsv->bhkv', k_dec, v_i)
        return new_state, o

    s0 = jnp.zeros((B, H, DK, DV), F32)
    _, o = lax.scan(step, s0, (qc, kc, vc, gc))
    return o.transpose(1, 0, 3, 2, 4).reshape(B, S, H, DV)


def _hybrid_mixer(x, w_in, fox_fgate_b, hgrn_fgate_b, lb, fox_out_g, hgrn_out_g, w_out):
    B, S, _ = x.shape
    proj = x @ w_in
    fq, fk, fv, ff, hq, hf, hi, hg = _split_cols(proj)
    fq = fq.reshape(B, S, FOX_HEADS, FOX_HEAD_DIM)
    fk = fk.reshape(B, S, FOX_HEADS, FOX_HEAD_DIM)
    fv = fv.reshape(B, S, FOX_HEADS, FOX_HEAD_DIM)
    fox_log_f = jax.nn.log_sigmoid((ff + fox_fgate_b).astype(F32))
    fox_o = _fox_attention(fq, fk, fv, fox_log_f)
    fox_o = _head_rms_norm(fox_o, fox_out_g.reshape(FOX_HEADS, FOX_HEAD_DIM), x.dtype)
    lb = lb.reshape(HGRN_HEADS, HGRN_EXPAND)
    z = (hf + hgrn_fgate_b).astype(F32).reshape(B, S, HGRN_HEADS, HGRN_EXPAND)
    h_log_f = jnp.logaddexp(jnp.log(lb), jnp.log1p(-lb) + jax.nn.log_sigmoid(z))
    h_k = -jnp.expm1(h_log_f)
    h_q = hq.astype(F32).reshape(B, S, HGRN_HEADS, HGRN_EXPAND)
    h_v = hi.astype(F32).reshape(B, S, HGRN_HEADS, HGRN_HEAD_V)
    h_o = _hgrn2_recurrence(h_q, h_k, h_v, h_log_f)
    h_gate = jax.nn.silu(hg.astype(F32).reshape(B, S, HGRN_HEADS, HGRN_HEAD_V))
    h_o = (_head_rms_norm(h_o, hgrn_out_g.reshape(HGRN_HEADS, HGRN_HEAD_V), F32) * h_gate).astype(x.dtype)
    mixed = jnp.concatenate([fox_o.reshape(B, S, FOX_WIDTH), h_o.reshape(B, S, HGRN_WIDTH)], axis=-1)
    return mixed @ w_out


def _peer_ffn(x, w_q, sub_keys, u_tab, v_tab):
    B, S, D = x.shape
    T = B * S
    xt = x.reshape(T, D)
    q = (xt @ w_q).astype(F32).reshape(T, PEER_HEADS, 2, PEER_HALF)
    s = jnp.einsum('thpd,hpnd->thpn', q, sub_keys.astype(F32))
    s_top, i_top = lax.top_k(s, PEER_TOPK)
    cand = s_top[:, :, 0, :, None] + s_top[:, :, 1, None, :]
    cand_idx = i_top[:, :, 0, :, None] * PEER_NKEYS + i_top[:, :, 1, None, :]
    cand = cand.reshape(T, PEER_HEADS, PEER_TOPK * PEER_TOPK)
    cand_idx = cand_idx.reshape(T, PEER_HEADS, PEER_TOPK * PEER_TOPK)
    best, pos = lax.top_k(cand, PEER_TOPK)
    idx = jnp.take_along_axis(cand_idx, pos, axis=-1)
    gates = jax.nn.softmax(best, axis=-1)
    E = PEER_HEADS * PEER_TOPK
    nblk = T // PEER_TOKEN_BLOCK

    def blk(args):
        x_b, idx_b, g_b = args
        u = u_tab[idx_b]
        h = jax.nn.gelu(jnp.einsum('td,ted->te', x_b, u), approximate=False) * g_b
        return jnp.einsum('te,ted->td', h, v_tab[idx_b])

    y = lax.map(blk, (xt.reshape(nblk, PEER_TOKEN_BLOCK, D),
                      idx.reshape(nblk, PEER_TOKEN_BLOCK, E),
                      gates.astype(x.dtype).reshape(nblk, PEER_TOKEN_BLOCK, E)))
    return y.reshape(B, S, D)


def setup_inputs(seed: int = 0) -> dict:
    key = jax.random.key(seed)
    ks = jax.random.split(key, 20)
    L = DEPTH
    nrm = jax.random.normal
    col_scale = jnp.concatenate([
        jnp.ones((2 * FOX_WIDTH,), F32),
        jnp.full((FOX_WIDTH,), DEEPNORM_BETA, F32),
        jnp.ones((FOX_HEADS + 2 * HGRN_KEY_WIDTH,), F32),
        jnp.full((HGRN_WIDTH,), DEEPNORM_BETA, F32),
        jnp.ones((HGRN_WIDTH,), F32)])
    return {
        "x": nrm(ks[0], (BATCH, SEQ, D_MODEL), F32),
        "ln_in_g": 1.0 + 0.02 * nrm(ks[1], (D_MODEL,), F32),
        "ln_in_b": 0.02 * nrm(ks[2], (D_MODEL,), F32),
        "w_in": nrm(ks[3], (L, D_MODEL, IN_WIDTH), F32) * (D_MODEL ** -0.5) * col_scale,
        "fox_fgate_b": FOX_FGATE_BIAS_INIT + 0.1 * nrm(ks[4], (L, FOX_HEADS), F32),
        "hgrn_fgate_b": 0.02 * nrm(ks[5], (L, HGRN_KEY_WIDTH), F32),
        "hgrn_lb_logits": 0.1 * nrm(ks[6], (DEPTH + 1, HGRN_KEY_WIDTH), F32),
        "fox_out_g": 1.0 + 0.02 * nrm(ks[7], (L, FOX_WIDTH), F32),
        "hgrn_out_g": 1.0 + 0.02 * nrm(ks[8], (L, HGRN_WIDTH), F32),
        "w_out": nrm(ks[9], (L, MIX_WIDTH, D_MODEL), F32) * (MIX_WIDTH ** -0.5) * DEEPNORM_BETA,
        "ln_mix_g": 1.0 + 0.02 * nrm(ks[10], (L, D_MODEL), F32),
        "ln_mix_b": 0.02 * nrm(ks[11], (L, D_MODEL), F32),
        "peer_w_q": nrm(ks[12], (L, D_MODEL, PEER_HEADS * PEER_QDIM), F32) * (D_MODEL ** -0.5),
        "peer_sub_keys": nrm(ks[13], (L, PEER_HEADS, 2, PEER_NKEYS, PEER_HALF), F32) * (PEER_HALF ** -0.5),
        "peer_u": nrm(ks[14], (L, PEER_NEXPERTS, D_MODEL), F32) * (D_MODEL ** -0.5) * DEEPNORM_BETA,
        "peer_v": nrm(ks[15], (L, PEER_NEXPERTS, D_MODEL), F32) * DEEPNORM_BETA,
        "ln_ffn_g": 1.0 + 0.02 * nrm(ks[16], (L, D_MODEL), F32),
        "ln_ffn_b": 0.02 * nrm(ks[17], (L, D_MODEL), F32),
    }


def reference(x, ln_in_g, ln_in_b, w_in, fox_fgate_b, hgrn_fgate_b, hgrn_lb_logits,
              fox_out_g, hgrn_out_g, w_out, ln_mix_g, ln_mix_b, peer_w_q, peer_sub_keys,
              peer_u, peer_v, ln_ffn_g, ln_ffn_b):
    x = _layer_norm(x, ln_in_g, ln_in_b)
    lb_all = jnp.cumsum(jax.nn.softmax(hgrn_lb_logits.astype(F32), axis=0), axis=0)
    for l in range(DEPTH):
        mix = _hybrid_mixer(x, w_in[l], fox_fgate_b[l], hgrn_fgate_b[l], lb_all[l],
                            fox_out_g[l], hgrn_out_g[l], w_out[l])
        x = _layer_norm(DEEPNORM_ALPHA * x + mix, ln_mix_g[l], ln_mix_b[l])
        ffn = _peer_ffn(x, peer_w_q[l], peer_sub_keys[l], peer_u[l], peer_v[l])
        x = _layer_norm(DEEPNORM_ALPHA * x + ffn, ln_ffn_g[l], ln_ffn_b[l])
    return x
```

```python
import os
from contextlib import ExitStack
import numpy as np
import concourse.bass as bass
import concourse.mybir as mybir
from concourse.bass_utils import run_bass_kernel_spmd

F32 = mybir.dt.float32; BF16 = mybir.dt.bfloat16; I32 = mybir.dt.int32; U32 = mybir.dt.uint32
ALU = mybir.AluOpType; AF = mybir.ActivationFunctionType; AX = mybir.AxisListType

SEQ = 4096; DM = 1024; NT = SEQ // 128
INW = 3592
ALPHA = 2.0 ** 0.25
LN_EPS = 1e-5; RMS_EPS = 1e-6
NEG = -1.0e30


class Trk:
    __slots__ = ("lw", "rd")
    def __init__(self):
        self.lw = None; self.rd = {}


class Sched:
    ENGS = ("pe", "act", "dve", "pool", "sp")
    def __init__(self, nc, es):
        self.nc = nc; self.es = es
        self.q = {e: [] for e in self.ENGS}
        self.cnt = {e: 0 for e in self.ENGS}
        self.waited = {e: {} for e in self.ENGS}
        self.sems = {}
        for e in self.ENGS:
            self.sems[e] = es.enter_context(nc.semaphore("s_" + e))
        self.dma_cnt = {}
        self.cap = None
        self.iter = 0
        self.stamp = {}
    def capture(self, f):
        self.cap = []
        f()
        lst = self.cap; self.cap = None
        return lst
    def _fresh(self, eng, reads, writes, lag):
        lim = self.iter - lag
        for t in list(reads) + list(writes):
            if t.lw and t.lw[0] != eng and self.stamp.get(t.lw, -10) > lim:
                return True
        for t in writes:
            for k, v in t.rd.items():
                if k != eng and self.stamp.get((k, v), -10) > lim:
                    return True
        return False
    def replay(self, lst, k, lag=0):
        n = 0
        while lst and n < k:
            it = lst[0]
            reads, writes = (it[3], it[4]) if it[0] == "op" else (it[4], it[5])
            if lag and self._fresh(it[1], reads, writes, lag):
                break
            lst.pop(0); n += 1
            if it[0] == "op": self.op(it[1], it[2], it[3], it[4])
            else: self.dma(it[1], it[2], it[3], it[4], it[5])
    def dsem(self, name):
        self.sems[name] = self.es.enter_context(self.nc.semaphore("d_" + name))
        self.dma_cnt[name] = 0
        return name
    def _deps(self, eng, reads, writes):
        deps = {}
        def add(k, v):
            if k == "pe" and eng == "pe":
                return
            if deps.get(k, 0) < v:
                deps[k] = v
        for t in reads:
            if t.lw: add(*t.lw)
        for t in writes:
            if t.lw: add(*t.lw)
            for k, v in t.rd.items(): add(k, v)
        w = self.waited[eng]
        for k, v in deps.items():
            if w.get(k, 0) < v:
                self.q[eng].append(("w", k, v)); w[k] = v
    def op(self, eng, fn, reads=(), writes=()):
        if self.cap is not None:
            self.cap.append(("op", eng, fn, list(reads), list(writes))); return
        self._deps(eng, reads, writes)
        self.cnt[eng] += 1; n = self.cnt[eng]
        self.stamp[(eng, n)] = self.iter
        self.q[eng].append(("o", fn, eng, 1))
        for t in reads:
            if t.rd.get(eng, 0) < n: t.rd[eng] = n
        for t in writes:
            t.lw = (eng, n); t.rd = {}
    def dma(self, eng, fn, sem, reads=(), writes=()):
        if self.cap is not None:
            self.cap.append(("dma", eng, fn, sem, list(reads), list(writes))); return
        self._deps(eng, reads, writes)
        self.dma_cnt[sem] += 16
        n = self.dma_cnt[sem]
        self.stamp[(sem, n)] = self.iter
        self.q[eng].append(("o", fn, sem, 16))
        for t in reads:
            if t.rd.get(sem, 0) < n: t.rd[sem] = n
        for t in writes:
            t.lw = (sem, n); t.rd = {}
    def barrier(self, engs=None):
        for e in (engs or self.ENGS):
            w = self.waited[e]
            for k in self.sems:
                v = self.cnt[k] if k in self.cnt else self.dma_cnt[k]
                if v > 0 and w.get(k, 0) < v and k != e:
                    self.q[e].append(("w", k, v)); w[k] = v
    def emit(self):
        sems = self.sems
        sig = {e: set() for e in self.ENGS}
        for e in self.ENGS:
            for it in self.q[e]:
                if it[0] == "w" and it[1] in sig:
                    sig[it[1]].add(it[2])
        if os.environ.get("KSIG", "") == "all":
            sig = {e: set(range(1, self.cnt[e] + 1)) for e in self.ENGS}
        rank = {e: {v: r + 1 for r, v in enumerate(sorted(sig[e]))} for e in self.ENGS}
        cnts = {e: 0 for e in self.ENGS}
        def run(e, name, items):
            n = 0
            for it in items:
                if it[0] == "w":
                    k, v = it[1], it[2]
                    e.wait_ge(sems[k], rank[k][v] if k in rank else v)
                else:
                    ins = it[1](e)
                    if it[2] in rank:
                        n += 1
                        if n in rank[it[2]]:
                            ins.then_inc(sems[it[2]], 1)
                    else:
                        ins.then_inc(sems[it[2]], it[3])
        with self.nc.Block() as block:
            @block.tensor
            def _(e): run(e, "pe", self.q["pe"])
            @block.scalar
            def _(e): run(e, "act", self.q["act"])
            @block.vector
            def _(e): run(e, "dve", self.q["dve"])
            @block.gpsimd
            def _(e): run(e, "pool", self.q["pool"])
            @block.sync
            def _(e): run(e, "sp", self.q["sp"])


class Arena:
    def __init__(self, aps):
        self.aps = aps
        self.off = {k: 0 for k in aps}
    def mark(self):
        return dict(self.off)
    def reset(self, m=None):
        self.off = dict(m) if m is not None else {k: 0 for k in self.aps}
    def alloc(self, shape, dt=F32):
        n = 1
        for s_ in shape[1:]: n *= s_
        ap, width = self.aps[dt]
        o = self.off[dt]
        if dt == BF16 and (n % 2):
            n_al = n + 1
        else:
            n_al = n
        assert o + n_al <= width, ("arena overflow", dt, o, n_al, width)
        a = ap[:, o:o + n]
        self.off[dt] = o + n_al
        if len(shape) > 2:
            names = ["a%d" % k for k in range(len(shape) - 1)]
            pat = "p (" + " ".join(names) + ") -> p " + " ".join(names)
            a = a.rearrange(pat, **{nm: s_ for nm, s_ in zip(names[:-1], shape[1:-1])})
        return a


def build_program(debug=False, stop=None):
    nc = bass.Bass("TRN2", target_bir_lowering=False)
    es = ExitStack()
    S = Sched(nc, es)
    op = S.op

    def din(name, shape, dt=F32):
        return nc.dram_tensor(name, shape, dt, kind="ExternalInput").ap()
    x_d = din("x", [SEQ, DM])
    ln_in_g = din("ln_in_g", [DM]); ln_in_b = din("ln_in_b", [DM])
    w_in = din("w_in", [DM, INW])
    fox_fgb = din("fox_fgate_b", [8])
    hfb_d = din("hgrn_fgate_b_fm", [128, 4])
    hlb_d = din("hgrn_lb_fm", [128, 8])
    fox_og = din("fox_out_g", [512]); hgrn_og = din("hgrn_out_g", [512])
    w_out = din("w_out", [DM, DM])
    ln_mix_g = din("ln_mix_g", [DM]); ln_mix_b = din("ln_mix_b", [DM])
    w_q = din("peer_w_q", [DM, 2048])
    keysT_d = din("keysT", [128, 16, 128])
    peer_u = din("peer_u", [16384, DM]); peer_v = din("peer_v", [16384, DM])
    ln_ffn_g = din("ln_ffn_g", [DM]); ln_ffn_b = din("ln_ffn_b", [DM])
    out_d = nc.dram_tensor("out", [SEQ, DM], F32, kind="ExternalOutput").ap()
    skind = "ExternalOutput" if debug else "Internal"
    xn_scr = nc.dram_tensor("xn_scr", [SEQ, DM], F32, kind=skind).ap()
    mix_scr = nc.dram_tensor("mix_scr", [SEQ, DM], BF16, kind=skind).ap()
    uv_scr = nc.dram_tensor("uv_scr", [16384, 2 * DM], BF16, kind="Internal").ap()
    def finish():
        S.barrier(["sp"])
        S.emit()
        es.close()
        return nc

    def sbt(name, shape, dt=F32):
        return es.enter_context(nc.sbuf_tensor(name, shape, dt))
    ident_f = sbt("ident_f", [128, 128]); ident_b = sbt("ident_b", [128, 128], BF16)
    t_const = Trk()
    WF, WB, WI = 16600, 63700, 640
    A = Arena({F32: (sbt("arena_f", [128, WF])[:], WF), BF16: (sbt("arena_b", [128, WB], BF16)[:], WB),
               I32: (sbt("arena_i", [128, 256], I32)[:], 256), U32: (sbt("arena_u", [128, WI], U32)[:], WI)})
    banks = [es.enter_context(nc.psum_tensor("bank%d" % b, [128, 512], F32)) for b in range(7)]
    tbank = es.enter_context(nc.psum_tensor("tbank", [128, 1024], BF16))
    tb_v = [tbank[:, 0:512].rearrange("p (c t) -> p c t", c=4), tbank[:, 512:1024].rearrange("p (c t) -> p c t", c=4)]
    _tbk = Trk()
    tbk_t = [_tbk, _tbk]

    op("pool", lambda e: e.memset(ident_f[:], 0.0), [], [t_const])
    op("pool", lambda e: e.affine_select(out=ident_f[:], in_=ident_f[:], pattern=[[-1, 128]], compare_op=ALU.not_equal,
                                         fill=1.0, base=0, channel_multiplier=1), [t_const], [t_const])
    op("pool", lambda e: e.tensor_copy(out=ident_b[:], in_=ident_f[:]), [t_const], [t_const])

    def wview(w_ap, c0, n):
        return w_ap.rearrange("(c p) n -> p c n", p=128)[:, :, c0:c0 + n]

    wst = [sbt("wst%d" % k, [128, 8, 128])[:] for k in range(2)]
    wst_t = [Trk(), Trk()]
    wst_s = [S.dsem("wst0"), S.dsem("wst1")]
    wst_n = [0]
    def load_w_bf16(dst, dst_t, w_ap, c0, ncols, scale=None):
        for k in range(0, ncols, 128):
            n = min(128, ncols - k)
            b = wst_n[0] % 2; wst_n[0] += 1
            S.dma("sp", lambda e, b=b, k=k, n=n: e.dma_start(out=wst[b][:, :, 0:n], in_=wview(w_ap, c0 + k, n)),
                  wst_s[b], writes=[wst_t[b]])
            if scale is None:
                if b == 0:
                    op("act", lambda e, b=b, k=k, n=n: e.copy(out=dst[:, :, k:k + n], in_=wst[b][:, :, 0:n]), [wst_t[b]], [dst_t])
                else:
                    op("pool", lambda e, b=b, k=k, n=n: e.tensor_copy(out=dst[:, :, k:k + n], in_=wst[b][:, :, 0:n]), [wst_t[b]], [dst_t])
            else:
                op("act", lambda e, b=b, k=k, n=n: e.mul(out=dst[:, :, k:k + n], in_=wst[b][:, :, 0:n], mul=scale), [wst_t[b]], [dst_t])

    par_chain = Trk()
    def bcast_load(dst, dst_t, vec_ap, sem):
        S.dma("sp", lambda e: e.dma_start(out=dst, in_=vec_ap.partition_broadcast(128)), sem, writes=[dst_t, par_chain])

    def layer_norm(src, src_trks, gb, bb, gb_t, dst32, dst32_t, dstbf, dstbf_t, tmp, gb_eng="pool"):
        st, mv, lnv, rstd, xh = tmp["st"], tmp["mv"], tmp["lnv"], tmp["rstd"], tmp["xh"]
        tt = tmp["t"]
        op("dve", lambda e: e.bn_stats(out=st[:, 0, :], in_=src[:, 0:512]), src_trks, [tt["st"]])
        op("dve", lambda e: e.bn_stats(out=st[:, 1, :], in_=src[:, 512:1024]), src_trks, [tt["st"]])
        op("dve", lambda e: e.bn_aggr(out=mv, in_=st.rearrange("p a b -> p (a b)")), [tt["st"]], [tt["mv"]])
        op("act", lambda e: e.activation(out=lnv, in_=mv[:, 1:2], func=AF.Ln, bias=LN_EPS, scale=1.0), [tt["mv"]], [tt["lnv"]])
        op("act", lambda e: e.activation(out=rstd, in_=lnv, func=AF.Exp, scale=-0.5), [tt["lnv"]], [tt["rstd"]])
        op("dve", lambda e: e.tensor_scalar(out=xh, in0=src, scalar1=mv[:, 0:1], scalar2=rstd[:, 0:1],
                                            op0=ALU.subtract, op1=ALU.mult), src_trks + [tt["mv"], tt["rstd"]], [tt["xh"]])
        op(gb_eng, lambda e: e.tensor_tensor(out=xh, in0=xh, in1=gb, op=ALU.mult), [tt["xh"], gb_t], [tt["xh"]])
        op(gb_eng, lambda e: e.tensor_tensor(out=dst32, in0=xh, in1=bb, op=ALU.add), [tt["xh"], gb_t], [dst32_t])
        if dstbf is not None:
            op("act", lambda e: e.copy(out=dstbf, in_=dst32), [dst32_t], [dstbf_t])

    def ln_tmp(Ar):
        d = {"st": Ar.alloc([128, 2, 6]), "mv": Ar.alloc([128, 2]), "lnv": Ar.alloc([128, 1]), "rstd": Ar.alloc([128, 1]),
             "xh": Ar.alloc([128, 1024])}
        d["t"] = {k: Trk() for k in ("st", "mv", "lnv", "rstd", "xh")}
        return d


    NST = 3
    stf = [A.alloc([128, DM]) for _ in range(NST)]; stf_t = [Trk() for _ in range(NST)]; stf_s = [S.dsem("stf%d" % k) for k in range(NST)]
    stb = [A.alloc([128, DM], BF16) for _ in range(NST)]; stb_t = [Trk() for _ in range(NST)]; stb_s = [S.dsem("stb%d" % k) for k in range(NST)]
    def _prepass():
        for t in range(256):
            tab = peer_u if t < 128 else peer_v
            r = t % 128; cb = 0 if t < 128 else DM
            k = t % NST
            S.dma("pool", lambda e, tab=tab, r=r, k=k: e.dma_start(out=stf[k], in_=tab[r * 128:(r + 1) * 128, :]), stf_s[k], writes=[stf_t[k]])
            op("dve", lambda e, k=k: e.tensor_copy(out=stb[k], in_=stf[k]), [stf_t[k]], [stb_t[k]])
            S.dma("pool", lambda e, r=r, cb=cb, k=k: e.dma_start(out=uv_scr[r * 128:(r + 1) * 128, cb:cb + DM], in_=stb[k]), stb_s[k], reads=[stb_t[k]])
    pp = S.capture(_prepass)
    if stop == 'P':
        S.replay(pp, len(pp))
        S.barrier()
        return finish()
    xnT = A.alloc([128, 8, SEQ], BF16)
    xnT_t = [Trk() for _ in range(NT)]
    mark_AB = A.mark()
    g1b = A.alloc([128, DM]); b1b = A.alloc([128, DM]); gb1_t = Trk()
    s_par = S.dsem("par")
    bcast_load(g1b, gb1_t, ln_in_g, s_par); bcast_load(b1b, gb1_t, ln_in_b, s_par)
    xt = [A.alloc([128, DM]) for _ in range(2)]; xt_t = [Trk(), Trk()]; xt_s = [S.dsem("xt0"), S.dsem("xt1")]
    xn32 = [A.alloc([128, DM]) for _ in range(2)]; xn32_t = [Trk(), Trk()]; xn32_s = [S.dsem("xn0"), S.dsem("xn1")]
    xnb = [A.alloc([128, DM], BF16) for _ in range(2)]; xnb_t = [Trk(), Trk()]
    lnA = ln_tmp(A)
    bank_t = [Trk() for _ in range(7)]
    for i in range(NT):
        b = i % 2
        S.replay(pp, 6)
        S.dma("sp", lambda e, i=i, b=b: e.dma_start(out=xt[b], in_=x_d[i * 128:(i + 1) * 128, :]), xt_s[b], writes=[xt_t[b]])
        layer_norm(xt[b], [xt_t[b]], g1b, b1b, gb1_t, xn32[b], xn32_t[b], xnb[b], xnb_t[b], lnA, gb_eng="dve")
        S.dma("sp", lambda e, i=i, b=b: e.dma_start(out=xn_scr[i * 128:(i + 1) * 128, :], in_=xn32[b]), xn32_s[b], reads=[xn32_t[b]])
        for half in range(2):
            pb = half
            pv = tb_v[pb]
            for c4 in range(4):
                c = half * 4 + c4
                op("pe", lambda e, pv=pv, c4=c4, c=c, b=b: e.transpose(out=pv[:, c4, :], in_=xnb[b][:, c * 128:(c + 1) * 128],
                                                                      identity=ident_b[:]), [xnb_t[b], t_const], [tbk_t[pb]])
            eng = "act" if half == 0 else "dve"
            if eng == "act":
                op("act", lambda e, pv=pv, half=half, i=i: e.copy(out=xnT[:, half * 4:half * 4 + 4, i * 128:(i + 1) * 128], in_=pv),
                   [tbk_t[pb]], [xnT_t[i]])
            else:
                op("dve", lambda e, pv=pv, half=half, i=i: e.tensor_copy(out=xnT[:, half * 4:half * 4 + 4, i * 128:(i + 1) * 128], in_=pv),
                   [tbk_t[pb]], [xnT_t[i]])
    if stop == 'A':
        S.barrier()
        return finish()

    fgb_b = A.alloc([128, 8]); fgb_t = Trk()
    bcast_load(fgb_b, fgb_t, fox_fgb, s_par)
    gfox = A.alloc([128, 512]); gfox_t = Trk()
    bcast_load(gfox, gfox_t, fox_og, s_par)
    triN = A.alloc([128, 256]); tri_t = Trk()
    op("pool", lambda e: e.memset(triN, -1.0), [], [tri_t])
    op("pool", lambda e: e.affine_select(out=triN[:, 0:128], in_=triN[:, 0:128], pattern=[[1, 128]], compare_op=ALU.is_ge,
                                         fill=0.0, base=0, channel_multiplier=-1), [tri_t], [tri_t])
    maskf = A.alloc([128, 128]); maskb = A.alloc([128, 128], BF16); mask_t = Trk()
    op("pool", lambda e: e.memset(maskf, 0.0), [], [mask_t])
    op("pool", lambda e: e.affine_select(out=maskf, in_=maskf, pattern=[[1, 128]], compare_op=ALU.is_ge,
                                         fill=-30000.0, base=0, channel_multiplier=-1), [mask_t], [mask_t])
    op("pool", lambda e: e.tensor_copy(out=maskb, in_=maskf), [mask_t], [mask_t])
    wff = A.alloc([128, 8, 8], BF16); wff_t = Trk()
    load_w_bf16(wff, wff_t, w_in, 1536, 8)
    cc = A.alloc([128, NT, 16]); cc_t = [Trk() for _ in range(NT)]
    zt = A.alloc([128, 8]); zt_t = Trk(); et = A.alloc([128, 8]); et_t = Trk(); lt = A.alloc([128, 8]); lt_t = Trk()
    for i in range(NT):
        pb = i % 2
        ffp = banks[pb][:, 0:8]; cps = banks[pb][:, 16:32]
        for c in range(8):
            op("pe", lambda e, c=c, i=i, ffp=ffp: e.matmul(ffp, lhsT=xnT[:, c, i * 128:(i + 1) * 128], rhs=wff[:, c, :],
                                                           start=(c == 0), stop=(c == 7)), [xnT_t[i], wff_t], [bank_t[pb]])
        op("dve", lambda e, ffp=ffp: e.tensor_tensor(out=zt, in0=ffp, in1=fgb_b, op=ALU.add), [bank_t[pb], fgb_t], [zt_t])
        op("act", lambda e: e.activation(out=et, in_=zt, func=AF.Exp, scale=-1.0), [zt_t], [et_t])
        op("act", lambda e: e.activation(out=lt, in_=et, func=AF.Ln, bias=1.0, scale=1.0), [et_t], [lt_t])
        op("pe", lambda e, cps=cps: e.matmul(cps[:, 0:8], lhsT=triN[:, 0:128], rhs=lt, start=True, stop=True), [tri_t, lt_t], [bank_t[pb]])
        op("pe", lambda e, cps=cps: e.matmul(cps[:, 8:16], lhsT=triN[:, 128:256], rhs=lt, start=True, stop=True), [tri_t, lt_t], [bank_t[pb]])
        if i == 0:
            op("dve", lambda e, cps=cps: e.tensor_copy(out=cc[:, 0, :], in_=cps), [bank_t[pb]], [cc_t[0]])
        else:
            op("dve", lambda e, cps=cps, i=i: e.tensor_tensor(out=cc[:, i, :].rearrange("p (a b) -> p a b", a=2),
                                                              in0=cps.rearrange("p (a b) -> p a b", a=2),
                                                              in1=cc[:, i - 1, 8:16].unsqueeze(1).to_broadcast([128, 2, 8]), op=ALU.add),
               [bank_t[pb], cc_t[i - 1]], [cc_t[i]])

    wq_b = A.alloc([128, 8, 128], BF16); wk_b = A.alloc([128, 8, 128], BF16); wv_b = A.alloc([128, 8, 128], BF16)
    wq_t = Trk(); wk_t = Trk(); wv_t = Trk()
    qT = A.alloc([128, SEQ], BF16); kT = A.alloc([128, SEQ], BF16); qT_t = [Trk() for _ in range(8)]; kT_t = [Trk() for _ in range(8)]
    vaug = A.alloc([128, NT, 2, 65], BF16); vaug_t = [Trk() for _ in range(NT)]
    op("pool", lambda e: e.memset(vaug[:, :, :, 64:65], 1.0), [], vaug_t)
    NPT = 6; LA = 3
    pT = [A.alloc([128, 128], BF16) for _ in range(NPT)]; pT_t = [Trk() for _ in range(NPT)]
    NBB = 4
    biasb = [A.alloc([128, NT]) for _ in range(NBB)]; biasb_t = [Trk() for _ in range(NBB)]
    rz = A.alloc([128, 2, 1]); rz_t = Trk(); on = A.alloc([128, 2, 64]); on_t = Trk(); junk64 = A.alloc([128, 64]); junk_t = Trk()
    ssq = A.alloc([128, 2]); ssq_t = Trk(); lnr = A.alloc([128, 2]); lnr_t = Trk(); rs2 = A.alloc([128, 2]); rs2_t = Trk()
    fo = [A.alloc([128, 128], BF16) for _ in range(2)]; fo_t = [Trk(), Trk()]; fo_s = [S.dsem("fo0"), S.dsem("fo1")]
    s_slots = [(banks[k][:, 0:128], bank_t[k]) for k in range(4)]
    o_acc = [(banks[4][:, 0:130].rearrange("p (h d) -> p h d", h=2), bank_t[4]), (banks[5][:, 0:130].rearrange("p (h d) -> p h d", h=2), bank_t[5])]
    epi_n = [0]
    for p in range(4):
        load_w_bf16(wq_b, wq_t, w_in, p * 128, 128, scale=0.125)
        load_w_bf16(wk_b, wk_t, w_in, 512 + p * 128, 128)
        load_w_bf16(wv_b, wv_t, w_in, 1024 + p * 128, 128)
        for tg in range(8):
            for (wb, wt, dst, dt_, pb) in ((wq_b, wq_t, qT, qT_t, 0), (wk_b, wk_t, kT, kT_t, 1)):
                for c in range(8):
                    op("pe", lambda e, wb=wb, c=c, tg=tg, pb=pb: e.matmul(banks[pb][:], lhsT=wb[:, c, :], rhs=xnT[:, c, tg * 512:(tg + 1) * 512],
                                                                          start=(c == 0), stop=(c == 7)),
                       [wt] + xnT_t[tg * 4:tg * 4 + 4], [bank_t[pb]])
                if pb == 0:
                    op("act", lambda e, dst=dst, tg=tg, pb=pb: e.copy(out=dst[:, tg * 512:(tg + 1) * 512], in_=banks[pb][:]), [bank_t[pb]], [dt_[tg]])
                else:
                    op("dve", lambda e, dst=dst, tg=tg, pb=pb: e.tensor_copy(out=dst[:, tg * 512:(tg + 1) * 512], in_=banks[pb][:]), [bank_t[pb]], [dt_[tg]])
        for i in range(NT):
            pb = i % 2
            for c in range(8):
                op("pe", lambda e, c=c, i=i, pb=pb: e.matmul(banks[pb][:, 0:128], lhsT=xnT[:, c, i * 128:(i + 1) * 128], rhs=wv_b[:, c, :],
                                                             start=(c == 0), stop=(c == 7)), [xnT_t[i], wv_t], [bank_t[pb]])
            op("act", lambda e, i=i, pb=pb: e.copy(out=vaug[:, i, :, 0:64], in_=banks[pb][:, 0:128].rearrange("p (h d) -> p h d", h=2)),
               [bank_t[pb]], [vaug_t[i]])
        blocks = [(i, h2, j) for i in range(NT) for h2 in range(2) for j in range(i + 1)]
        nb = len(blocks)
        cur_bias = {}
        for n in range(nb + LA):
            if n % 10 == 0:
                S.replay(pp, 1)
            if n < nb:
                i, h2, j = blocks[n]
                h = 2 * p + h2
                if j == 0:
                    bb_i = (2 * i + h2) % NBB
                    cur_bias[(i, h2)] = bb_i
                    op("dve", lambda e, i=i, h=h, bb_i=bb_i: e.tensor_scalar(out=biasb[bb_i][:, 0:i + 1], in0=cc[:, 0:i + 1, h],
                                                                               scalar1=cc[:, i, 8 + h:9 + h], scalar2=-1.0,
                                                                               op0=ALU.subtract, op1=ALU.mult),
                       cc_t[0:i + 1], [biasb_t[bb_i]])
                bb_i = cur_bias[(i, h2)]
                sl, sl_t = s_slots[n % 4]
                r0 = h2 * 64
                op("pe", lambda e, sl=sl, r0=r0, i=i, j=j: e.matmul(sl, lhsT=kT[r0:r0 + 64, j * 128:(j + 1) * 128], rhs=qT[r0:r0 + 64, i * 128:(i + 1) * 128],
                                                                   start=True, stop=(j != i)), [kT_t[j // 4], qT_t[i // 4]], [sl_t])
                if j == i:
                    op("pe", lambda e, sl=sl: e.matmul(sl, lhsT=ident_b[:], rhs=maskb, start=False, stop=True), [t_const, mask_t], [sl_t])
                pb_ = n % NPT
                op("act", lambda e, sl=sl, pb_=pb_, bb_i=bb_i, j=j: e.activation(out=pT[pb_], in_=sl, func=AF.Exp, bias=biasb[bb_i][:, j:j + 1], scale=1.0),
                   [sl_t, biasb_t[bb_i]], [pT_t[pb_]])
            m = n - LA
            if m >= 0:
                i, h2, j = blocks[m]
                oa, oa_t = o_acc[i % 2]
                pb_ = m % NPT
                op("pe", lambda e, oa=oa, h2=h2, pb_=pb_, i=i, j=j: e.matmul(oa[:, h2, :], lhsT=pT[pb_], rhs=vaug[:, j, h2, :],
                                                                            start=(j == 0), stop=(j == i)), [pT_t[pb_], vaug_t[j]], [oa_t])
                if h2 == 1 and j == i:
                    fb = epi_n[0] % 2; epi_n[0] += 1
                    op("dve", lambda e, oa=oa: e.reciprocal(out=rz, in_=oa[:, :, 64:65]), [oa_t], [rz_t])
                    op("dve", lambda e, oa=oa: e.tensor_tensor(out=on, in0=oa[:, :, 0:64], in1=rz.to_broadcast([128, 2, 64]), op=ALU.mult),
                       [oa_t, rz_t], [on_t])
                    for hh in range(2):
                        op("dve", lambda e, hh=hh: e.scalar_tensor_tensor(out=junk64, in0=on[:, hh, :], scalar=1.0, in1=on[:, hh, :],
                                                                          op0=ALU.mult, op1=ALU.mult, accum_out=ssq[:, hh:hh + 1]),
                           [on_t], [junk_t, ssq_t])
                    op("act", lambda e: e.activation(out=lnr, in_=ssq, func=AF.Ln, bias=RMS_EPS, scale=1.0 / 64.0), [ssq_t], [lnr_t])
                    op("act", lambda e: e.activation(out=rs2, in_=lnr, func=AF.Exp, scale=-0.5), [lnr_t], [rs2_t])
                    op("dve", lambda e: e.tensor_tensor(out=on, in0=on, in1=rs2.unsqueeze(2).to_broadcast([128, 2, 64]), op=ALU.mult),
                       [on_t, rs2_t], [on_t])
                    op("dve", lambda e, fb=fb, p=p: e.tensor_tensor(out=fo[fb].rearrange("p (h d) -> p h d", h=2), in0=on,
                                                                    in1=gfox[:, p * 128:(p + 1) * 128].rearrange("p (h d) -> p h d", h=2), op=ALU.mult),
                       [on_t, gfox_t], [fo_t[fb]])
                    S.dma("sp", lambda e, fb=fb, i=i, p=p: e.dma_start(out=mix_scr[i * 128:(i + 1) * 128, p * 128:(p + 1) * 128], in_=fo[fb]),
                          fo_s[fb], reads=[fo_t[fb]])
    S.barrier()
    if stop == 'B1':
        return finish()

    A.reset(mark_AB)
    hfb = A.alloc([128, 4]); hlb = A.alloc([128, 8]); hp_t = Trk()
    S.dma("sp", lambda e: e.dma_start(out=hfb, in_=hfb_d), s_par, writes=[hp_t, par_chain])
    S.dma("sp", lambda e: e.dma_start(out=hlb, in_=hlb_d), s_par, writes=[hp_t, par_chain])
    nfb = A.alloc([128, 4]); lbv = A.alloc([128, 4]); oml = A.alloc([128, 4]); tl = A.alloc([128, 4]); hq_t = Trk()
    op("dve", lambda e: e.tensor_scalar(out=nfb, in0=hfb, scalar1=-1.0, scalar2=None, op0=ALU.mult), [hp_t], [hq_t])
    op("dve", lambda e: e.tensor_tensor(out=tl, in0=hlb[:, 0:4], in1=hlb[:, 4:8], op=ALU.subtract), [hp_t], [hq_t])
    op("act", lambda e: e.activation(out=tl, in_=tl, func=AF.Exp, scale=-1.0), [hq_t], [hq_t])
    op("dve", lambda e: e.tensor_scalar(out=tl, in0=tl, scalar1=1.0, scalar2=None, op0=ALU.add), [hq_t], [hq_t])
    op("dve", lambda e: e.reciprocal(out=lbv, in_=tl), [hq_t], [hq_t])
    op("dve", lambda e: e.tensor_scalar(out=oml, in0=lbv, scalar1=-1.0, scalar2=1.0, op0=ALU.mult, op1=ALU.add), [hq_t], [hq_t])
    ghg = A.alloc([128, 512]); ghg_t = Trk()
    bcast_load(ghg, ghg_t, hgrn_og, s_par)
    cmask = A.alloc([128, 512]); mA = A.alloc([128, 512]); mB = A.alloc([128, 512]); cm_t = Trk()
    op("pool", lambda e: e.memset(cmask, 1.0), [], [cm_t])
    op("pool", lambda e: e.memset(cmask.rearrange("p (c t) -> p c t", t=64)[:, :, 0:1], 0.0), [cm_t], [cm_t])
    op("pool", lambda e: e.memset(mA, 0.0), [], [cm_t])
    op("pool", lambda e: e.memset(mA.rearrange("p (c t) -> p c t", t=128)[:, :, 0:64], 1.0), [cm_t], [cm_t])
    op("pool", lambda e: e.memset(mB, 0.0), [], [cm_t])
    op("pool", lambda e: e.memset(mB.rearrange("p (c t) -> p c t", t=128)[:, :, 64:128], 1.0), [cm_t], [cm_t])
    bdm = A.alloc([128, 128]); bdm_t = Trk()
    op("pool", lambda e: e.memset(bdm, 1.0), [], [bdm_t])
    op("pool", lambda e: e.affine_select(out=bdm, in_=bdm, pattern=[[1, 128]], compare_op=ALU.is_ge, fill=0.0, base=0, channel_multiplier=-1),
       [bdm_t], [bdm_t])
    op("pool", lambda e: e.memset(bdm[0:64, 64:128], 0.0), [bdm_t], [bdm_t])
    whq = A.alloc([128, 8, 128], BF16); whf = A.alloc([128, 8, 128], BF16); whi = A.alloc([128, 8, 128], BF16); whg = A.alloc([128, 8, 128], BF16)
    whq_t = Trk(); whf_t = Trk(); whi_t = Trk(); whg_t = Trk()
    v_tok = [A.alloc([128, 4, 128], BF16) for _ in range(2)]; v_t = [[Trk() for _ in range(4)] for _ in range(2)]
    gg = [A.alloc([128, 4, 128]) for _ in range(2)]; gg_t = [[Trk() for _ in range(4)] for _ in range(2)]
    t_e = A.alloc([128, 512]); t_f = A.alloc([128, 512]); t_g = A.alloc([128, 512]); t_k = A.alloc([128, 512]); t_b = A.alloc([128, 512])
    t_x = A.alloc([128, 512]); t_e2 = A.alloc([128, 512]); t_d = A.alloc([128, 512])
    te_t, tf_t, tg_t, tk_t, tb_t, tx_t, te2_t, td_t = (Trk() for _ in range(8))
    qtf = [A.alloc([128, 512], BF16) for _ in range(2)]; qtl = [A.alloc([128, 512], BF16) for _ in range(2)]; qth = [A.alloc([128, 512], BF16) for _ in range(2)]
    ktT = [A.alloc([128, 512], BF16) for _ in range(2)]; kdT = [A.alloc([128, 512], BF16) for _ in range(2)]
    kd_tok = [A.alloc([128, 4, 128], BF16) for _ in range(2)]
    ebl = [A.alloc([128, 8]) for _ in range(2)]
    qtf_t = [Trk() for _ in range(2)]; qtl_t = [Trk() for _ in range(2)]; qth_t = [Trk() for _ in range(2)]; ktT_t = [Trk() for _ in range(2)]
    kdT_t = [Trk() for _ in range(2)]; kdk_t = [Trk() for _ in range(2)]; ebl_t = [Trk() for _ in range(2)]
    Sf = A.alloc([128, 128]); Sf_t = Trk()
    Sb = [A.alloc([128, 128], BF16) for _ in range(2)]; Sb_t = [Trk(), Trk()]
    sm = [A.alloc([128, 128], BF16) for _ in range(2)]; sm_t = [Trk(), Trk()]
    o_sb = [A.alloc([128, 128]) for _ in range(2)]; osb_t = [Trk(), Trk()]
    junk128 = A.alloc([128, 128]); j128_t = Trk()
    hss = [A.alloc([128, 1]) for _ in range(2)]; hln = [A.alloc([128, 1]) for _ in range(2)]; hrs = [A.alloc([128, 1]) for _ in range(2)]
    hss_t = [Trk(), Trk()]; hln_t = [Trk(), Trk()]; hrs_t = [Trk(), Trk()]
    yb = [A.alloc([128, 128], BF16) for _ in range(2)]; yb_t = [Trk(), Trk()]; yb_s = [S.dsem("yb0"), S.dsem("yb1")]
    silu_t = A.alloc([128, 128]); silu_tt = Trk()
    sc_sl = [banks[4][:, 0:128]] * 2; sc_t = [bank_t[4]] * 2
    o_sl = [banks[5][:, 0:128]] * 2; o_t = [bank_t[5]] * 2
    ds_sl = [banks[6][:, k * 128:(k + 1) * 128] for k in range(2)]; ds_t = [bank_t[6]] * 2
    pv_sl = [banks[2][:, 0:128], banks[3][:, 0:128]]; pv_t = [bank_t[2], bank_t[3]]
    epi2 = [0]
    for h in range(4):
        load_w_bf16(whq, whq_t, w_in, 1544 + h * 128, 128); load_w_bf16(whf, whf_t, w_in, 2056 + h * 128, 128)
        load_w_bf16(whi, whi_t, w_in, 2568 + h * 128, 128); load_w_bf16(whg, whg_t, w_in, 3080 + h * 128, 128)
        op("dve", lambda e: e.memset(Sf, 0.0), [], [Sf_t])
        op("pool", lambda e: e.memset(Sb[0], 0.0), [], [Sb_t[0]])
        hc = slice(h * 128, (h + 1) * 128)
        def front(tg, h=h, hc=hc):
            d = tg % 2
            xs = xnT_t[tg * 4:tg * 4 + 4]
            for tt in range(4):
                tok = (tg * 4 + tt) * 128
                for c in range(8):
                    op("pe", lambda e, c=c, tok=tok: e.matmul(pv_sl[0], lhsT=xnT[:, c, tok:tok + 128], rhs=whi[:, c, :], start=(c == 0), stop=(c == 7)),
                       [xnT_t[tg * 4 + tt], whi_t], [pv_t[0]])
                op("act", lambda e, tt=tt, d=d: e.copy(out=v_tok[d][:, tt, :], in_=pv_sl[0]), [pv_t[0]], [v_t[d][tt]])
                for c in range(8):
                    op("pe", lambda e, c=c, tok=tok: e.matmul(pv_sl[1], lhsT=xnT[:, c, tok:tok + 128], rhs=whg[:, c, :], start=(c == 0), stop=(c == 7)),
                       [xnT_t[tg * 4 + tt], whg_t], [pv_t[1]])
                op("act", lambda e: e.activation(out=silu_t, in_=pv_sl[1], func=AF.Silu), [pv_t[1]], [silu_tt])
                op("dve", lambda e, tt=tt, d=d, hc=hc: e.tensor_tensor(out=gg[d][:, tt, :], in0=silu_t, in1=ghg[:, hc], op=ALU.mult), [silu_tt, ghg_t], [gg_t[d][tt]])
            for (wb, wt, pb) in ((whq, whq_t, 0), (whf, whf_t, 1)):
                for c in range(8):
                    op("pe", lambda e, wb=wb, c=c, pb=pb, tg=tg: e.matmul(banks[pb][:], lhsT=wb[:, c, :],
                                                                          rhs=xnT[:, c, tg * 512:(tg + 1) * 512], start=(c == 0), stop=(c == 7)),
                       [wt] + xs, [bank_t[pb]])
            op("act", lambda e, h=h: e.activation(out=t_f, in_=banks[1][:], func=AF.Sigmoid, bias=hfb[:, h:h + 1], scale=1.0), [bank_t[1], hp_t], [tf_t])
            op("dve", lambda e, h=h: e.tensor_scalar(out=t_f, in0=t_f, scalar1=oml[:, h:h + 1], scalar2=lbv[:, h:h + 1], op0=ALU.mult, op1=ALU.add),
               [tf_t, hq_t], [tf_t])
            op("act", lambda e: e.activation(out=t_g, in_=t_f, func=AF.Ln), [tf_t], [tg_t])
            op("dve", lambda e: e.tensor_scalar(out=t_k, in0=t_f, scalar1=-1.0, scalar2=1.0, op0=ALU.mult, op1=ALU.add), [tf_t], [tk_t])
            op("dve", lambda e: e.tensor_tensor_scan(out=t_b, data0=cmask, data1=t_g, initial=0.0, op0=ALU.mult, op1=ALU.add), [cm_t, tg_t], [tb_t])
            op("act", lambda e: e.activation(out=t_e, in_=t_b, func=AF.Exp), [tb_t], [te_t])
            op("dve", lambda e: e.tensor_tensor(out=t_x, in0=banks[0][:], in1=t_e, op=ALU.mult), [bank_t[0], te_t], [tx_t])
            op("act", lambda e, d=d: e.copy(out=qtf[d], in_=t_x), [tx_t], [qtf_t[d]])
            op("dve", lambda e, d=d: e.tensor_tensor(out=qtl[d], in0=t_x, in1=mA, op=ALU.mult), [tx_t, cm_t], [qtl_t[d]])
            op("dve", lambda e, d=d: e.tensor_tensor(out=qth[d], in0=t_x, in1=mB, op=ALU.mult), [tx_t, cm_t], [qth_t[d]])
            op("act", lambda e: e.activation(out=t_e2, in_=t_b, func=AF.Exp, scale=-1.0), [tb_t], [te2_t])
            op("dve", lambda e, d=d: e.tensor_tensor(out=ktT[d], in0=t_k, in1=t_e2, op=ALU.mult), [tk_t, te2_t], [ktT_t[d]])
            tb3 = t_b.rearrange("p (c t) -> p c t", t=64)
            op("dve", lambda e, tb3=tb3: e.tensor_tensor(out=t_d.rearrange("p (c t) -> p c t", t=64), in0=tb3[:, :, 63:64].to_broadcast([128, 8, 64]),
                                                          in1=tb3, op=ALU.subtract), [tb_t], [td_t])
            op("act", lambda e: e.activation(out=t_d, in_=t_d, func=AF.Exp), [td_t], [td_t])
            op("dve", lambda e, d=d: e.tensor_tensor(out=kdT[d], in0=t_k, in1=t_d, op=ALU.mult), [tk_t, td_t], [kdT_t[d]])
            op("act", lambda e, d=d, tb3=tb3: e.activation(out=ebl[d].unsqueeze(2), in_=tb3[:, :, 63:64], func=AF.Exp), [tb_t], [ebl_t[d]])
            pv = tb_v[d]
            for tt in range(4):
                op("pe", lambda e, pv=pv, tt=tt, d=d: e.transpose(out=pv[:, tt, :], in_=kdT[d][:, tt * 128:(tt + 1) * 128], identity=ident_b[:]),
                   [kdT_t[d], t_const], [tbk_t[d]])
            op("dve", lambda e, pv=pv, d=d: e.tensor_copy(out=kd_tok[d], in_=pv), [tbk_t[d]], [kdk_t[d]])
        def chunks(tg, deferred, h=h, hc=hc):
            d = tg % 2
            def opr(*a):
                op(*a)
                S.iter += 1
                S.replay(deferred, 2, lag=1)
            for tt in range(4):
                S.replay(pp, 3)
                tile_i = tg * 4 + tt
                cs = slice(tt * 128, (tt + 1) * 128)
                k2 = tt % 2
                opr("pe", lambda e, d=d, cs=cs, k2=k2: e.matmul(sc_sl[k2], lhsT=ktT[d][:, cs], rhs=qtf[d][:, cs], start=True, stop=True),
                   [ktT_t[d], qtf_t[d]], [sc_t[k2]])
                opr("dve", lambda e, k2=k2: e.tensor_tensor(out=sm[k2], in0=sc_sl[k2], in1=bdm, op=ALU.mult), [sc_t[k2], bdm_t], [sm_t[k2]])
                opr("pe", lambda e, k2=k2, tt=tt, d=d: e.matmul(o_sl[k2], lhsT=sm[k2], rhs=v_tok[d][:, tt, :], start=True, stop=False),
                   [sm_t[k2], v_t[d][tt]], [o_t[k2]])
                opr("pe", lambda e, k2=k2, cs=cs, d=d: e.matmul(o_sl[k2], lhsT=qtl[d][:, cs], rhs=Sb[0], start=False, stop=False),
                   [qtl_t[d], Sb_t[0]], [o_t[k2]])
                opr("pe", lambda e, tt=tt, d=d: e.matmul(ds_sl[0], lhsT=kd_tok[d][0:64, tt, :], rhs=v_tok[d][0:64, tt, :], start=True, stop=True),
                   [kdk_t[d], v_t[d][tt]], [ds_t[0]])
                opr("dve", lambda e, tt=tt, d=d: e.scalar_tensor_tensor(out=Sf, in0=Sf, scalar=ebl[d][:, 2 * tt:2 * tt + 1], in1=ds_sl[0],
                                                                       op0=ALU.mult, op1=ALU.add), [Sf_t, ebl_t[d], ds_t[0]], [Sf_t])
                opr("act", lambda e: e.copy(out=Sb[1], in_=Sf), [Sf_t], [Sb_t[1]])
                opr("pe", lambda e, k2=k2, cs=cs, d=d: e.matmul(o_sl[k2], lhsT=qth[d][:, cs], rhs=Sb[1], start=False, stop=True),
                   [qth_t[d], Sb_t[1]], [o_t[k2]])
                opr("pe", lambda e, tt=tt, d=d: e.matmul(ds_sl[1], lhsT=kd_tok[d][64:128, tt, :], rhs=v_tok[d][64:128, tt, :], start=True, stop=True),
                   [kdk_t[d], v_t[d][tt]], [ds_t[1]])
                opr("dve", lambda e, tt=tt, d=d: e.scalar_tensor_tensor(out=Sf, in0=Sf, scalar=ebl[d][:, 2 * tt + 1:2 * tt + 2], in1=ds_sl[1],
                                                                       op0=ALU.mult, op1=ALU.add), [Sf_t, ebl_t[d], ds_t[1]], [Sf_t])
                opr("act", lambda e: e.copy(out=Sb[0], in_=Sf), [Sf_t], [Sb_t[0]])
                eb = epi2[0] % 2; epi2[0] += 1
                opr("act", lambda e, k2=k2, eb=eb: e.copy(out=o_sb[eb], in_=o_sl[k2]), [o_t[k2]], [osb_t[eb]])
                opr("dve", lambda e, eb=eb: e.scalar_tensor_tensor(out=junk128, in0=o_sb[eb], scalar=1.0, in1=o_sb[eb], op0=ALU.mult, op1=ALU.mult,
                                                                  accum_out=hss[eb]), [osb_t[eb]], [j128_t, hss_t[eb]])
                opr("act", lambda e, eb=eb: e.activation(out=hln[eb], in_=hss[eb], func=AF.Ln, bias=RMS_EPS, scale=1.0 / 128.0), [hss_t[eb]], [hln_t[eb]])
                opr("act", lambda e, eb=eb: e.activation(out=hrs[eb], in_=hln[eb], func=AF.Exp, scale=-0.5), [hln_t[eb]], [hrs_t[eb]])
                opr("dve", lambda e, eb=eb, tt=tt, d=d: e.scalar_tensor_tensor(out=yb[eb], in0=o_sb[eb], scalar=hrs[eb][:, 0:1], in1=gg[d][:, tt, :],
                                                                              op0=ALU.mult, op1=ALU.mult), [osb_t[eb], hrs_t[eb], gg_t[d][tt]], [yb_t[eb]])
                S.dma("sp", lambda e, eb=eb, tile_i=tile_i, h=h: e.dma_start(out=mix_scr[tile_i * 128:(tile_i + 1) * 128, 512 + h * 128:512 + (h + 1) * 128],
                                                                            in_=yb[eb]), yb_s[eb], reads=[yb_t[eb]])
        front(0)
        for tg in range(8):
            deferred = S.capture(lambda: front(tg + 1)) if tg + 1 < 8 else []
            chunks(tg, deferred)
            S.replay(deferred, len(deferred))
    S.replay(pp, len(pp))
    S.barrier()
    if stop == 'B2':
        return finish()

    A.reset()
    wqp = A.alloc([128, 8, 2048], BF16); wqp_t = Trk()
    wo = A.alloc([128, 8, 1024], BF16); wo_t = Trk()
    kTb = A.alloc([128, 16, 128], BF16); kTb_t = Trk()
    load_w_bf16(wo, wo_t, w_out, 0, 1024)
    load_w_bf16(wqp, wqp_t, w_q, 0, 2048)
    for k in range(2):
        b = wst_n[0] % 2; wst_n[0] += 1
        S.dma("sp", lambda e, b=b, k=k: e.dma_start(out=wst[b], in_=keysT_d[:, k * 8:(k + 1) * 8, :]), wst_s[b], writes=[wst_t[b]])
        op("act", lambda e, b=b, k=k: e.copy(out=kTb[:, k * 8:(k + 1) * 8, :], in_=wst[b]), [wst_t[b]], [kTb_t])
    g2b = A.alloc([128, DM]); b2b = A.alloc([128, DM]); g3b = A.alloc([128, DM]); b3b = A.alloc([128, DM]); gb2_t = Trk(); gb3_t = Trk()
    bcast_load(g2b, gb2_t, ln_mix_g, s_par); bcast_load(b2b, gb2_t, ln_mix_b, s_par)
    bcast_load(g3b, gb3_t, ln_ffn_g, s_par); bcast_load(b3b, gb3_t, ln_ffn_b, s_par)
    iota16 = A.alloc([128, 16]); io_t = Trk()
    op("pool", lambda e: e.iota(iota16, pattern=[[1, 16]], base=0, channel_multiplier=0, allow_small_or_imprecise_dtypes=True), [], [io_t])
    NG = 12
    gbuf = [A.alloc([128, 2 * DM], BF16) for _ in range(NG)]; gbuf_t = [Trk() for _ in range(NG)]; gbuf_s = [S.dsem("g%d" % k) for k in range(NG)]
    mt = A.alloc([128, DM], BF16); mt_t = Trk(); mt_s = S.dsem("mt")
    mT = A.alloc([128, 8, 128], BF16); mT_t = Trk()
    xnt = [A.alloc([128, DM]) for _ in range(2)]; xnt_t = [Trk(), Trk()]; xnt_s = [S.dsem("xnt0"), S.dsem("xnt1")]
    xn2 = [A.alloc([128, DM]) for _ in range(2)]; xn2_t = [Trk(), Trk()]
    idx = [A.alloc([128, 128], I32) for _ in range(2)]; idx_t = [Trk(), Trk()]
    gates = [A.alloc([128, 128]) for _ in range(2)]; gate_t = [Trk(), Trk()]
    ot_s = [S.dsem("ot0"), S.dsem("ot1")]
    xn2b = [A.alloc([128, DM], BF16) for _ in range(2)]; xn2b_t = [Trk(), Trk()]
    x2T = A.alloc([128, 8, 128], BF16); x2T_t = Trk()
    qTs = A.alloc([128, 16, 128], BF16); qTs_t = Trk()
    s_sb = A.alloc([128, 2048]); ssb_t = Trk()
    sR = A.alloc([128, 128]); sR_t = Trk()
    v16 = A.alloc([128, 8, 2, 16]); v16_t = Trk(); i16 = A.alloc([128, 8, 2, 16], U32); i16_t = Trk()
    cand = A.alloc([128, 8, 256]); cand_t = Trk(); cR = A.alloc([128, 256]); cR_t = Trk()
    b16 = A.alloc([128, 8, 16]); b16_t = Trk(); pos = A.alloc([128, 8, 16], U32); pos_t = Trk()
    pa = A.alloc([128, 8, 16], U32); pbq = A.alloc([128, 8, 16], U32); paf = A.alloc([128, 8, 16]); pbf = A.alloc([128, 8, 16]); dec_t = Trk()
    i16f = A.alloc([128, 8, 2, 16]); i16f_t = Trk()
    eq = cand.rearrange("p h (a b) -> p h a b", a=16); eq_t = cand_t
    If = A.alloc([128, 8, 16]); Jf = A.alloc([128, 8, 16]); IJ_t = Trk()
    gd = A.alloc([128, 8, 16]); gsum = A.alloc([128, 8]); gd_t = Trk()
    GS = 1
    xu = A.alloc([128, 128]); xu_t = [Trk() for _ in range(128 // GS)]; hg = A.alloc([128, 128]); hg_t = [Trk() for _ in range(128 // GS)]
    hf = A.alloc([128, 128]); hf_t = [Trk() for _ in range(128 // GS)]
    NPR = 4
    prod = [A.alloc([128, DM], BF16) for _ in range(NPR)]; prod_t = [Trk() for _ in range(NPR)]; prc = [0]
    NDG = 3
    dg = [A.alloc([128, 2, 128], BF16) for _ in range(NDG)]; dg_t = [Trk() for _ in range(NDG)]; dgc = [0]
    lnC = ln_tmp(A)
    lnD = lnC
    gcount = [0]

    def pre(i):
        q = i % 2
        rows = slice(i * 128, (i + 1) * 128)
        S.dma("sp", lambda e: e.dma_start(out=mt, in_=mix_scr[rows, :]), mt_s, writes=[mt_t])
        S.dma("sp", lambda e: e.dma_start(out=xnt[q], in_=xn_scr[rows, :]), xnt_s[q], writes=[xnt_t[q]])
        for half in range(2):
            pv = tb_v[half]
            for c4 in range(4):
                c = half * 4 + c4
                op("pe", lambda e, pv=pv, c4=c4, c=c: e.transpose(out=pv[:, c4, :], in_=mt[:, c * 128:(c + 1) * 128], identity=ident_b[:]),
                   [mt_t, t_const], [tbk_t[half]])
            op("act", lambda e, pv=pv, half=half: e.copy(out=mT[:, half * 4:half * 4 + 4, :], in_=pv), [tbk_t[half]], [mT_t])
        for nh in range(2):
            for c in range(8):
                op("pe", lambda e, nh=nh, c=c: e.matmul(banks[nh][:], lhsT=mT[:, c, :], rhs=wo[:, c, nh * 512:(nh + 1) * 512], start=(c == 0), stop=(c == 7)),
                   [mT_t, wo_t], [bank_t[nh]])
            op("dve", lambda e, nh=nh: e.scalar_tensor_tensor(out=xnt[q][:, nh * 512:(nh + 1) * 512], in0=xnt[q][:, nh * 512:(nh + 1) * 512], scalar=ALPHA,
                                                              in1=banks[nh][:], op0=ALU.mult, op1=ALU.add), [xnt_t[q], bank_t[nh]], [xnt_t[q]])
        layer_norm(xnt[q], [xnt_t[q]], g2b, b2b, gb2_t, xn2[q], xn2_t[q], xn2b[q], xn2b_t[q], lnC, gb_eng="dve")
        for half in range(2):
            pv = tb_v[half]
            for c4 in range(4):
                c = half * 4 + c4
                op("pe", lambda e, pv=pv, c4=c4, c=c: e.transpose(out=pv[:, c4, :], in_=xn2b[q][:, c * 128:(c + 1) * 128], identity=ident_b[:]),
                   [xn2b_t[q], t_const], [tbk_t[half]])
            op("act", lambda e, pv=pv, half=half: e.copy(out=x2T[:, half * 4:half * 4 + 4, :], in_=pv), [tbk_t[half]], [x2T_t])
        for c4g in range(4):
            for cq in range(4):
                cidx = c4g * 4 + cq
                for k in range(8):
                    op("pe", lambda e, cq=cq, cidx=cidx, k=k: e.matmul(banks[6][:, cq * 128:(cq + 1) * 128], lhsT=wqp[:, k, cidx * 128:(cidx + 1) * 128],
                                                                      rhs=x2T[:, k, :], start=(k == 0), stop=(k == 7)), [wqp_t, x2T_t], [bank_t[6]])
            op("act", lambda e, c4g=c4g: e.copy(out=qTs[:, c4g * 4:c4g * 4 + 4, :], in_=banks[6][:].rearrange("p (c t) -> p c t", c=4)),
               [bank_t[6]], [qTs_t])
        for half2 in range(2):
            for c8 in range(8):
                cidx = half2 * 8 + c8
                bk = 2 + c8 // 4
                op("pe", lambda e, cidx=cidx, bk=bk: e.matmul(banks[bk][:, (cidx % 4) * 128:(cidx % 4 + 1) * 128], lhsT=qTs[:, cidx, :], rhs=kTb[:, cidx, :],
                                                              start=True, stop=True), [qTs_t, kTb_t], [bank_t[bk]])
            for bq in range(2):
                op("act", lambda e, bq=bq, half2=half2: e.copy(out=s_sb[:, (half2 * 2 + bq) * 512:(half2 * 2 + bq + 1) * 512], in_=banks[2 + bq][:]),
                   [bank_t[2 + bq]], [ssb_t])
        for h in range(8):
            for pp in range(2):
                sl = s_sb[:, (2 * h + pp) * 128:(2 * h + pp + 1) * 128]
                op("dve", lambda e, sl=sl, h=h, pp=pp: e.max(out=v16[:, h, pp, 0:8], in_=sl), [ssb_t], [v16_t])
                op("dve", lambda e, sl=sl, h=h, pp=pp: e.max_index(out=i16[:, h, pp, 0:8], in_max=v16[:, h, pp, 0:8], in_values=sl), [ssb_t, v16_t], [i16_t])
                op("dve", lambda e, sl=sl, h=h, pp=pp: e.match_replace(out=sR, in_to_replace=v16[:, h, pp, 0:8], in_values=sl, imm_value=NEG),
                   [ssb_t, v16_t], [sR_t])
                op("dve", lambda e, h=h, pp=pp: e.max(out=v16[:, h, pp, 8:16], in_=sR), [sR_t], [v16_t])
                op("dve", lambda e, h=h, pp=pp: e.max_index(out=i16[:, h, pp, 8:16], in_max=v16[:, h, pp, 8:16], in_values=sR), [sR_t, v16_t], [i16_t])
            ch = cand[:, h, :]
            op("dve", lambda e, h=h, ch=ch: e.tensor_tensor(out=ch.rearrange("p (a b) -> p a b", a=16), in0=v16[:, h, 0, :].unsqueeze(2).to_broadcast([128, 16, 16]),
                                                            in1=v16[:, h, 1, :].unsqueeze(1).to_broadcast([128, 16, 16]), op=ALU.add), [v16_t], [cand_t])
            op("dve", lambda e, h=h, ch=ch: e.max(out=b16[:, h, 0:8], in_=ch), [cand_t], [b16_t])
            op("dve", lambda e, h=h, ch=ch: e.max_index(out=pos[:, h, 0:8], in_max=b16[:, h, 0:8], in_values=ch), [cand_t, b16_t], [pos_t])
            op("dve", lambda e, h=h, ch=ch: e.match_replace(out=cR, in_to_replace=b16[:, h, 0:8], in_values=ch, imm_value=NEG), [cand_t, b16_t], [cR_t])
            op("dve", lambda e, h=h: e.max(out=b16[:, h, 8:16], in_=cR), [cR_t], [b16_t])
            op("dve", lambda e, h=h: e.max_index(out=pos[:, h, 8:16], in_max=b16[:, h, 8:16], in_values=cR), [cR_t, b16_t], [pos_t])
        op("dve", lambda e: e.tensor_single_scalar(out=pa, in_=pos, scalar=4, op=ALU.logical_shift_right), [pos_t], [dec_t])
        op("dve", lambda e: e.tensor_single_scalar(out=pbq, in_=pos, scalar=15, op=ALU.bitwise_and), [pos_t], [dec_t])
        op("dve", lambda e: e.tensor_copy(out=paf, in_=pa), [dec_t], [dec_t])
        op("dve", lambda e: e.tensor_copy(out=pbf, in_=pbq), [dec_t], [dec_t])
        op("dve", lambda e: e.tensor_copy(out=i16f, in_=i16), [i16_t], [i16f_t])
        io4 = iota16.unsqueeze(1).unsqueeze(1).to_broadcast([128, 4, 16, 16])
        for (src, half, dst) in ((paf, 0, If), (pbf, 1, Jf)):
            for hh_ in range(2):
                hs = slice(hh_ * 4, hh_ * 4 + 4)
                op("dve", lambda e, src=src, hs=hs: e.tensor_tensor(out=eq[:, hs], in0=src[:, hs].unsqueeze(3).to_broadcast([128, 4, 16, 16]), in1=io4, op=ALU.is_equal),
                   [dec_t, io_t], [eq_t])
                op("dve", lambda e, half=half, hs=hs: e.tensor_tensor(out=eq[:, hs], in0=eq[:, hs], in1=i16f[:, hs, half, :].unsqueeze(2).to_broadcast([128, 4, 16, 16]), op=ALU.mult),
                   [eq_t, i16f_t], [eq_t])
                op("dve", lambda e, dst=dst, hs=hs: e.tensor_reduce(out=dst[:, hs], in_=eq[:, hs], axis=AX.X, op=ALU.add), [eq_t], [IJ_t])
        op("dve", lambda e: e.scalar_tensor_tensor(out=idx[q].rearrange("p (h k) -> p h k", h=8), in0=If, scalar=128.0, in1=Jf, op0=ALU.mult, op1=ALU.add),
           [IJ_t], [idx_t[q]])
        op("dve", lambda e: e.tensor_tensor(out=gd, in0=b16, in1=b16[:, :, 0:1].to_broadcast([128, 8, 16]), op=ALU.subtract), [b16_t], [gd_t])
        op("act", lambda e: e.activation(out=gd, in_=gd, func=AF.Exp), [gd_t], [gd_t])
        op("dve", lambda e: e.tensor_reduce(out=gsum, in_=gd, axis=AX.X, op=ALU.add), [gd_t], [gd_t])
        op("dve", lambda e: e.reciprocal(out=gsum, in_=gsum), [gd_t], [gd_t])
        op("dve", lambda e: e.tensor_tensor(out=gates[q].rearrange("p (h k) -> p h k", h=8), in0=gd, in1=gsum.unsqueeze(2).to_broadcast([128, 8, 16]), op=ALU.mult),
           [gd_t], [gate_t[q]])

    def gather(i, m):
        q = i % 2
        g = gcount[0] % NG; gcount[0] += 1
        S.dma("pool", lambda e: e.indirect_dma_start(out=gbuf[g], out_offset=None, in_=uv_scr[:, :],
                                                      in_offset=bass.IndirectOffsetOnAxis(ap=idx[q][:, m:m + 1], axis=0)),
              gbuf_s[g], reads=[idx_t[q]], writes=[gbuf_t[g]])
        return g

    def post_a(i):
        q = i % 2
        for nh in range(2):
            op("dve", lambda e, nh=nh: e.scalar_tensor_tensor(out=xnt[q][:, nh * 512:(nh + 1) * 512], in0=xn2[q][:, nh * 512:(nh + 1) * 512], scalar=ALPHA,
                                                              in1=banks[4 + nh][:], op0=ALU.mult, op1=ALU.add), [xn2_t[q], bank_t[4 + nh]], [xnt_t[q]])
    def post_b(i):
        q = i % 2
        rows = slice(i * 128, (i + 1) * 128)
        layer_norm(xnt[q], [xnt_t[q]], g3b, b3b, gb3_t, xnt[q], xnt_t[q], None, None, lnD, gb_eng="dve")
        S.dma("sp", lambda e: e.dma_start(out=out_d[rows, :], in_=xnt[q]), ot_s[q], reads=[xnt_t[q]])

    if os.environ.get("KDBG"):
        print("arena use phase C:", {str(k): v for k, v in A.off.items()})
    pre(0)
    for i in range(NT):
        q = i % 2
        def _defer():
            if i >= 1: post_b(i - 1)
            if i + 1 < NT: pre(i + 1)
        deferred = S.capture(_defer)
        per = max(1, -(-len(deferred) // 115))
        gof = {}
        def st1(m, q=q, i=i):
            g = gather(i, m); gof[m] = g
            pk = prc[0] % NPR; prc[0] += 1
            op("dve", lambda e: e.tensor_tensor(out=prod[pk], in0=gbuf[g][:, 0:DM], in1=xn2b[q], op=ALU.mult), [gbuf_t[g], xn2b_t[q]], [prod_t[pk]])
            op("act", lambda e: e.activation(out=prod[pk], in_=prod[pk], func=AF.Copy, accum_out=xu[:, m:m + 1]), [prod_t[pk]], [prod_t[pk], xu_t[m // 2]])
        def st2a(p):
            cs = slice(2 * p, 2 * p + 2)
            op("act", lambda e: e.activation(out=hg[:, cs], in_=xu[:, cs], func=AF.Gelu), [xu_t[p]], [hg_t[p]])
        def st2b(p, q=q):
            cs = slice(2 * p, 2 * p + 2)
            op("dve", lambda e: e.tensor_tensor(out=hf[:, cs], in0=hg[:, cs], in1=gates[q][:, cs], op=ALU.mult), [hg_t[p], gate_t[q]], [hf_t[p]])
        def st3(p):
            d_ = dgc[0] % NDG; dgc[0] += 1
            op("dve", lambda e: e.tensor_tensor(out=dg[d_], in0=ident_b[:].unsqueeze(1).to_broadcast([128, 2, 128]),
                                                in1=hf[:, 2 * p:2 * p + 2].unsqueeze(2).to_broadcast([128, 2, 128]), op=ALU.mult), [hf_t[p], t_const], [dg_t[d_]])
            for k_ in range(2):
                m = 2 * p + k_
                g = gof[m]
                for nh in range(2):
                    op("pe", lambda e, nh=nh, k_=k_, g=g, m=m: e.matmul(banks[4 + nh][:], lhsT=dg[d_][:, k_, :], rhs=gbuf[g][:, DM + nh * 512:DM + (nh + 1) * 512],
                                                                       start=(m == 0), stop=(m == 127)), [dg_t[d_], gbuf_t[g]], [bank_t[4 + nh]])
        for m in range(128 + 5):
            if m >= 4 and (m - 4) % 2 == 0 and (m - 4) // 2 < 64: st3((m - 4) // 2)
            if m >= 3 and (m - 3) % 2 == 0 and (m - 3) // 2 < 64: st2b((m - 3) // 2)
            if m >= 2 and (m - 2) % 2 == 0 and (m - 2) // 2 < 64: st2a((m - 2) // 2)
            if m < 128: st1(m)
            S.iter += 1
            S.replay(deferred, per + 1, lag=2)
        S.replay(deferred, len(deferred))
        post_a(i)
    post_b(NT - 1)
    S.barrier(["sp"])
    S.emit()
    es.close()
    return nc


_CACHE = {}


def kernel(**inputs):
    x = np.ascontiguousarray(np.asarray(inputs["x"], dtype=np.float32))
    f = lambda k: np.ascontiguousarray(np.asarray(inputs[k], dtype=np.float32))
    shared = {
        "ln_in_g": f("ln_in_g"), "ln_in_b": f("ln_in_b"),
        "w_in": f("w_in")[0],
        "fox_fgate_b": f("fox_fgate_b")[0],
        "hgrn_fgate_b_fm": np.ascontiguousarray(f("hgrn_fgate_b")[0].reshape(4, 128).T),
        "hgrn_lb_fm": np.ascontiguousarray(f("hgrn_lb_logits").reshape(2, 4, 128).transpose(2, 0, 1).reshape(128, 8)),
        "fox_out_g": f("fox_out_g")[0], "hgrn_out_g": f("hgrn_out_g")[0],
        "w_out": f("w_out")[0],
        "ln_mix_g": f("ln_mix_g")[0], "ln_mix_b": f("ln_mix_b")[0],
        "peer_w_q": f("peer_w_q")[0],
        "keysT": np.ascontiguousarray(f("peer_sub_keys")[0].reshape(16, 128, 128).transpose(2, 0, 1)),
        "peer_u": f("peer_u")[0], "peer_v": f("peer_v")[0],
        "ln_ffn_g": f("ln_ffn_g")[0], "ln_ffn_b": f("ln_ffn_b")[0],
    }
    if "nc" not in _CACHE:
        _CACHE["nc"] = build_program()
    nc = _CACHE["nc"]
    in_maps = []
    for c in range(8):
        m = dict(shared); m["x"] = x[c]
        in_maps.append(m)
    res = run_bass_kernel_spmd(nc, in_maps, core_ids=list(range(8)))
    out = np.stack([np.asarray(r["out"], dtype=np.float32).reshape(SEQ, DM) for r in res.results], axis=0)
    return out
```

```python
import os
from contextlib import ExitStack
import numpy as np
import concourse.bass as bass
import concourse.mybir as mybir
from concourse.bass_utils import run_bass_kernel_spmd

F32 = mybir.dt.float32; BF16 = mybir.dt.bfloat16; I32 = mybir.dt.int32; U32 = mybir.dt.uint32
ALU = mybir.AluOpType; AF = mybir.ActivationFunctionType; AX = mybir.AxisListType

SEQ = 4096; DM = 1024; NT = SEQ // 128
INW = 3592
ALPHA = 2.0 ** 0.25
LN_EPS = 1e-5; RMS_EPS = 1e-6
NEG = -1.0e30


class Trk:
    __slots__ = ("lw", "rd")
    def __init__(self):
        self.lw = None; self.rd = {}


class Sched:
    ENGS = ("pe", "act", "dve", "pool", "sp")
    def __init__(self, nc, es):
        self.nc = nc; self.es = es
        self.q = {e: [] for e in self.ENGS}
        self.cnt = {e: 0 for e in self.ENGS}
        self.waited = {e: {} for e in self.ENGS}
        self.sems = {}
        for e in self.ENGS:
            self.sems[e] = es.enter_context(nc.semaphore("s_" + e))
        self.dma_cnt = {}
        self.cap = None
        self.iter = 0
        self.stamp = {}
    def capture(self, f):
        self.cap = []
        f()
        lst = self.cap; self.cap = None
        return lst
    def _fresh(self, eng, reads, writes, lag):
        lim = self.iter - lag
        for t in list(reads) + list(writes):
            if t.lw and t.lw[0] != eng and self.stamp.get(t.lw, -10) > lim:
                return True
        for t in writes:
            for k, v in t.rd.items():
                if k != eng and self.stamp.get((k, v), -10) > lim:
                    return True
        return False
    def replay(self, lst, k, lag=0):
        n = 0
        while lst and n < k:
            it = lst[0]
            reads, writes = (it[3], it[4]) if it[0] == "op" else (it[4], it[5])
            if lag and self._fresh(it[1], reads, writes, lag):
                break
            lst.pop(0); n += 1
            if it[0] == "op": self.op(it[1], it[2], it[3], it[4])
            else: self.dma(it[1], it[2], it[3], it[4], it[5])
    def dsem(self, name):
        self.sems[name] = self.es.enter_context(self.nc.semaphore("d_" + name))
        self.dma_cnt[name] = 0
        return name
    def _deps(self, eng, reads, writes):
        deps = {}
        def add(k, v):
            if k == "pe" and eng == "pe":
                return
            if deps.get(k, 0) < v:
                deps[k] = v
        for t in reads:
            if t.lw: add(*t.lw)
        for t in writes:
            if t.lw: add(*t.lw)
            for k, v in t.rd.items(): add(k, v)
        w = self.waited[eng]
        for k, v in deps.items():
            if w.get(k, 0) < v:
                self.q[eng].append(("w", k, v)); w[k] = v
    def op(self, eng, fn, reads=(), writes=()):
        if self.cap is not None:
            self.cap.append(("op", eng, fn, list(reads), list(writes))); return
        self._deps(eng, reads, writes)
        self.cnt[eng] += 1; n = self.cnt[eng]
        self.stamp[(eng, n)] = self.iter
        self.q[eng].append(("o", fn, eng, 1))
        for t in reads:
            if t.rd.get(eng, 0) < n: t.rd[eng] = n
        for t in writes:
            t.lw = (eng, n); t.rd = {}
    def dma(self, eng, fn, sem, reads=(), writes=()):
        if self.cap is not None:
            self.cap.append(("dma", eng, fn, sem, list(reads), list(writes))); return
        self._deps(eng, reads, writes)
        self.dma_cnt[sem] += 16
        n = self.dma_cnt[sem]
        self.stamp[(sem, n)] = self.iter
        self.q[eng].append(("o", fn, sem, 16))
        for t in reads:
            if t.rd.get(sem, 0) < n: t.rd[sem] = n
        for t in writes:
            t.lw = (sem, n); t.rd = {}
    def barrier(self, engs=None):
        for e in (engs or self.ENGS):
            w = self.waited[e]
            for k in self.sems:
                v = self.cnt[k] if k in self.cnt else self.dma_cnt[k]
                if v > 0 and w.get(k, 0) < v and k != e:
                    self.q[e].append(("w", k, v)); w[k] = v
    def emit(self):
        sems = self.sems
        sig = {e: set() for e in self.ENGS}
        for e in self.ENGS:
            for it in self.q[e]:
                if it[0] == "w" and it[1] in sig:
                    sig[it[1]].add(it[2])
        if os.environ.get("KSIG", "") == "all":
            sig = {e: set(range(1, self.cnt[e] + 1)) for e in self.ENGS}
        rank = {e: {v: r + 1 for r, v in enumerate(sorted(sig[e]))} for e in self.ENGS}
        cnts = {e: 0 for e in self.ENGS}
        def run(e, name, items):
            n = 0
            for it in items:
                if it[0] == "w":
                    k, v = it[1], it[2]
                    e.wait_ge(sems[k], rank[k][v] if k in rank else v)
                else:
                    ins = it[1](e)
                    if it[2] in rank:
                        n += 1
                        if n in rank[it[2]]:
                            ins.then_inc(sems[it[2]], 1)
                    else:
                        ins.then_inc(sems[it[2]], it[3])
        with self.nc.Block() as block:
            @block.tensor
            def _(e): run(e, "pe", self.q["pe"])
            @block.scalar
            def _(e): run(e, "act", self.q["act"])
            @block.vector
            def _(e): run(e, "dve", self.q["dve"])
            @block.gpsimd
            def _(e): run(e, "pool", self.q["pool"])
            @block.sync
            def _(e): run(e, "sp", self.q["sp"])


class Arena:
    def __init__(self, aps):
        self.aps = aps
        self.off = {k: 0 for k in aps}
    def mark(self):
        return dict(self.off)
    def reset(self, m=None):
        self.off = dict(m) if m is not None else {k: 0 for k in self.aps}
    def alloc(self, shape, dt=F32):
        n = 1
        for s_ in shape[1:]: n *= s_
        ap, width = self.aps[dt]
        o = self.off[dt]
        if dt == BF16 and (n % 2):
            n_al = n + 1
        else:
            n_al = n
        assert o + n_al <= width, ("arena overflow", dt, o, n_al, width)
        a = ap[:, o:o + n]
        self.off[dt] = o + n_al
        if len(shape) > 2:
            names = ["a%d" % k for k in range(len(shape) - 1)]
            pat = "p (" + " ".join(names) + ") -> p " + " ".join(names)
            a = a.rearrange(pat, **{nm: s_ for nm, s_ in zip(names[:-1], shape[1:-1])})
        return a


def build_program(debug=False, stop=None):
    nc = bass.Bass("TRN2", target_bir_lowering=False)
    es = ExitStack()
    S = Sched(nc, es)
    op = S.op

    def din(name, shape, dt=F32):
        return nc.dram_tensor(name, shape, dt, kind="ExternalInput").ap()
    x_d = din("x", [SEQ, DM])
    ln_in_g = din("ln_in_g", [DM]); ln_in_b = din("ln_in_b", [DM])
    w_in = din("w_in", [DM, INW])
    fox_fgb = din("fox_fgate_b", [8])
    hfb_d = din("hgrn_fgate_b_fm", [128, 4])
    hlb_d = din("hgrn_lb_fm", [128, 8])
    fox_og = din("fox_out_g", [512]); hgrn_og = din("hgrn_out_g", [512])
    w_out = din("w_out", [DM, DM])
    ln_mix_g = din("ln_mix_g", [DM]); ln_mix_b = din("ln_mix_b", [DM])
    w_q = din("peer_w_q", [DM, 2048])
    keysT_d = din("keysT", [128, 16, 128])
    peer_u = din("peer_u", [16384, DM]); peer_v = din("peer_v", [16384, DM])
    ln_ffn_g = din("ln_ffn_g", [DM]); ln_ffn_b = din("ln_ffn_b", [DM])
    out_d = nc.dram_tensor("out", [SEQ, DM], F32, kind="ExternalOutput").ap()
    skind = "ExternalOutput" if debug else "Internal"
    xn_scr = nc.dram_tensor("xn_scr", [SEQ, DM], F32, kind=skind).ap()
    mix_scr = nc.dram_tensor("mix_scr", [SEQ, DM], BF16, kind=skind).ap()
    uv_scr = nc.dram_tensor("uv_scr", [16384, 2 * DM], BF16, kind="Internal").ap()
    def finish():
        S.barrier(["sp"])
        S.emit()
        es.close()
        return nc

    def sbt(name, shape, dt=F32):
        return es.enter_context(nc.sbuf_tensor(name, shape, dt))
    ident_f = sbt("ident_f", [128, 128]); ident_b = sbt("ident_b", [128, 128], BF16)
    t_const = Trk()
    WF, WB, WI = 16600, 63700, 640
    A = Arena({F32: (sbt("arena_f", [128, WF])[:], WF), BF16: (sbt("arena_b", [128, WB], BF16)[:], WB),
               I32: (sbt("arena_i", [128, 256], I32)[:], 256), U32: (sbt("arena_u", [128, WI], U32)[:], WI)})
    banks = [es.enter_context(nc.psum_tensor("bank%d" % b, [128, 512], F32)) for b in range(7)]
    tbank = es.enter_context(nc.psum_tensor("tbank", [128, 1024], BF16))
    tb_v = [tbank[:, 0:512].rearrange("p (c t) -> p c t", c=4), tbank[:, 512:1024].rearrange("p (c t) -> p c t", c=4)]
    _tbk = Trk()
    tbk_t = [_tbk, _tbk]

    op("pool", lambda e: e.memset(ident_f[:], 0.0), [], [t_const])
    op("pool", lambda e: e.affine_select(out=ident_f[:], in_=ident_f[:], pattern=[[-1, 128]], compare_op=ALU.not_equal,
                                         fill=1.0, base=0, channel_multiplier=1), [t_const], [t_const])
    op("pool", lambda e: e.tensor_copy(out=ident_b[:], in_=ident_f[:]), [t_const], [t_const])

    def wview(w_ap, c0, n):
        return w_ap.rearrange("(c p) n -> p c n", p=128)[:, :, c0:c0 + n]

    wst = [sbt("wst%d" % k, [128, 8, 128])[:] for k in range(2)]
    wst_t = [Trk(), Trk()]
    wst_s = [S.dsem("wst0"), S.dsem("wst1")]
    wst_n = [0]
    def load_w_bf16(dst, dst_t, w_ap, c0, ncols, scale=None):
        for k in range(0, ncols, 128):
            n = min(128, ncols - k)
            b = wst_n[0] % 2; wst_n[0] += 1
            S.dma("sp", lambda e, b=b, k=k, n=n: e.dma_start(out=wst[b][:, :, 0:n], in_=wview(w_ap, c0 + k, n)),
                  wst_s[b], writes=[wst_t[b]])
            if scale is None:
                if b == 0:
                    op("act", lambda e, b=b, k=k, n=n: e.copy(out=dst[:, :, k:k + n], in_=wst[b][:, :, 0:n]), [wst_t[b]], [dst_t])
                else:
                    op("dve", lambda e, b=b, k=k, n=n: e.tensor_copy(out=dst[:, :, k:k + n], in_=wst[b][:, :, 0:n]), [wst_t[b]], [dst_t])
            else:
                op("act", lambda e, b=b, k=k, n=n: e.mul(out=dst[:, :, k:k + n], in_=wst[b][:, :, 0:n], mul=scale), [wst_t[b]], [dst_t])

    par_chain = Trk()
    def bcast_load(dst, dst_t, vec_ap, sem):
        S.dma("sp", lambda e: e.dma_start(out=dst, in_=vec_ap.partition_broadcast(128)), sem, writes=[dst_t, par_chain])

    def layer_norm(src, src_trks, gb, bb, gb_t, dst32, dst32_t, dstbf, dstbf_t, tmp, gb_eng="pool"):
        st, mv, lnv, rstd, xh = tmp["st"], tmp["mv"], tmp["lnv"], tmp["rstd"], tmp["xh"]
        tt = tmp["t"]
        op("dve", lambda e: e.bn_stats(out=st[:, 0, :], in_=src[:, 0:512]), src_trks, [tt["st"]])
        op("dve", lambda e: e.bn_stats(out=st[:, 1, :], in_=src[:, 512:1024]), src_trks, [tt["st"]])
        op("dve", lambda e: e.bn_aggr(out=mv, in_=st.rearrange("p a b -> p (a b)")), [tt["st"]], [tt["mv"]])
        op("act", lambda e: e.activation(out=lnv, in_=mv[:, 1:2], func=AF.Ln, bias=LN_EPS, scale=1.0), [tt["mv"]], [tt["lnv"]])
        op("act", lambda e: e.activation(out=rstd, in_=lnv, func=AF.Exp, scale=-0.5), [tt["lnv"]], [tt["rstd"]])
        op("dve", lambda e: e.tensor_scalar(out=xh, in0=src, scalar1=mv[:, 0:1], scalar2=rstd[:, 0:1],
                                            op0=ALU.subtract, op1=ALU.mult), src_trks + [tt["mv"], tt["rstd"]], [tt["xh"]])
        op(gb_eng, lambda e: e.tensor_tensor(out=xh, in0=xh, in1=gb, op=ALU.mult), [tt["xh"], gb_t], [tt["xh"]])
        op(gb_eng, lambda e: e.tensor_tensor(out=dst32, in0=xh, in1=bb, op=ALU.add), [tt["xh"], gb_t], [dst32_t])
        if dstbf is not None:
            op("act", lambda e: e.copy(out=dstbf, in_=dst32), [dst32_t], [dstbf_t])

    def ln_tmp(Ar):
        d = {"st": Ar.alloc([128, 2, 6]), "mv": Ar.alloc([128, 2]), "lnv": Ar.alloc([128, 1]), "rstd": Ar.alloc([128, 1]),
             "xh": Ar.alloc([128, 1024])}
        d["t"] = {k: Trk() for k in ("st", "mv", "lnv", "rstd", "xh")}
        return d


    NST = 3
    stf = [A.alloc([128, DM]) for _ in range(NST)]; stf_t = [Trk() for _ in range(NST)]; stf_s = [S.dsem("stf%d" % k) for k in range(NST)]
    stb = [A.alloc([128, DM], BF16) for _ in range(NST)]; stb_t = [Trk() for _ in range(NST)]; stb_s = [S.dsem("stb%d" % k) for k in range(NST)]
    def _prepass():
        for t in range(256):
            tab = peer_u if t < 128 else peer_v
            r = t % 128; cb = 0 if t < 128 else DM
            k = t % NST
            S.dma("pool", lambda e, tab=tab, r=r, k=k: e.dma_start(out=stf[k], in_=tab[r * 128:(r + 1) * 128, :]), stf_s[k], writes=[stf_t[k]])
            op("dve", lambda e, k=k: e.tensor_copy(out=stb[k], in_=stf[k]), [stf_t[k]], [stb_t[k]])
            S.dma("pool", lambda e, r=r, cb=cb, k=k: e.dma_start(out=uv_scr[r * 128:(r + 1) * 128, cb:cb + DM], in_=stb[k]), stb_s[k], reads=[stb_t[k]])
    pp = S.capture(_prepass)
    if stop == 'P':
        S.replay(pp, len(pp))
        S.barrier()
        return finish()
    xnT = A.alloc([128, 8, SEQ], BF16)
    xnT_t = [Trk() for _ in range(NT)]
    mark_AB = A.mark()
    g1b = A.alloc([128, DM]); b1b = A.alloc([128, DM]); gb1_t = Trk()
    s_par = S.dsem("par")
    bcast_load(g1b, gb1_t, ln_in_g, s_par); bcast_load(b1b, gb1_t, ln_in_b, s_par)
    xt = [A.alloc([128, DM]) for _ in range(2)]; xt_t = [Trk(), Trk()]; xt_s = [S.dsem("xt0"), S.dsem("xt1")]
    xn32 = [A.alloc([128, DM]) for _ in range(2)]; xn32_t = [Trk(), Trk()]; xn32_s = [S.dsem("xn0"), S.dsem("xn1")]
    xnb = [A.alloc([128, DM], BF16) for _ in range(2)]; xnb_t = [Trk(), Trk()]
    lnA = ln_tmp(A)
    bank_t = [Trk() for _ in range(7)]
    for i in range(NT):
        b = i % 2
        S.replay(pp, 6)
        S.dma("sp", lambda e, i=i, b=b: e.dma_start(out=xt[b], in_=x_d[i * 128:(i + 1) * 128, :]), xt_s[b], writes=[xt_t[b]])
        layer_norm(xt[b], [xt_t[b]], g1b, b1b, gb1_t, xn32[b], xn32_t[b], xnb[b], xnb_t[b], lnA, gb_eng="dve")
        S.dma("sp", lambda e, i=i, b=b: e.dma_start(out=xn_scr[i * 128:(i + 1) * 128, :], in_=xn32[b]), xn32_s[b], reads=[xn32_t[b]])
        for half in range(2):
            pb = half
            pv = tb_v[pb]
            for c4 in range(4):
                c = half * 4 + c4
                op("pe", lambda e, pv=pv, c4=c4, c=c, b=b: e.transpose(out=pv[:, c4, :], in_=xnb[b][:, c * 128:(c + 1) * 128],
                                                                      identity=ident_b[:]), [xnb_t[b], t_const], [tbk_t[pb]])
            eng = "act" if half == 0 else "dve"
            if eng == "act":
                op("act", lambda e, pv=pv, half=half, i=i: e.copy(out=xnT[:, half * 4:half * 4 + 4, i * 128:(i + 1) * 128], in_=pv),
                   [tbk_t[pb]], [xnT_t[i]])
            else:
                op("dve", lambda e, pv=pv, half=half, i=i: e.tensor_copy(out=xnT[:, half * 4:half * 4 + 4, i * 128:(i + 1) * 128], in_=pv),
                   [tbk_t[pb]], [xnT_t[i]])
    if stop == 'A':
        S.barrier()
        return finish()

    fgb_b = A.alloc([128, 8]); fgb_t = Trk()
    bcast_load(fgb_b, fgb_t, fox_fgb, s_par)
    gfox = A.alloc([128, 512]); gfox_t = Trk()
    bcast_load(gfox, gfox_t, fox_og, s_par)
    triN = A.alloc([128, 256]); tri_t = Trk()
    op("pool", lambda e: e.memset(triN, -1.0), [], [tri_t])
    op("pool", lambda e: e.affine_select(out=triN[:, 0:128], in_=triN[:, 0:128], pattern=[[1, 128]], compare_op=ALU.is_ge,
                                         fill=0.0, base=0, channel_multiplier=-1), [tri_t], [tri_t])
    maskf = A.alloc([128, 128]); maskb = A.alloc([128, 128], BF16); mask_t = Trk()
    op("pool", lambda e: e.memset(maskf, 0.0), [], [mask_t])
    op("pool", lambda e: e.affine_select(out=maskf, in_=maskf, pattern=[[1, 128]], compare_op=ALU.is_ge,
                                         fill=-30000.0, base=0, channel_multiplier=-1), [mask_t], [mask_t])
    op("pool", lambda e: e.tensor_copy(out=maskb, in_=maskf), [mask_t], [mask_t])
    wff = A.alloc([128, 8, 8], BF16); wff_t = Trk()
    load_w_bf16(wff, wff_t, w_in, 1536, 8)
    cc = A.alloc([128, NT, 16]); cc_t = [Trk() for _ in range(NT)]
    zt = A.alloc([128, 8]); zt_t = Trk(); et = A.alloc([128, 8]); et_t = Trk(); lt = A.alloc([128, 8]); lt_t = Trk()
    for i in range(NT):
        pb = i % 2
        ffp = banks[pb][:, 0:8]; cps = banks[pb][:, 16:32]
        for c in range(8):
            op("pe", lambda e, c=c, i=i, ffp=ffp: e.matmul(ffp, lhsT=xnT[:, c, i * 128:(i + 1) * 128], rhs=wff[:, c, :],
                                                           start=(c == 0), stop=(c == 7)), [xnT_t[i], wff_t], [bank_t[pb]])
        op("dve", lambda e, ffp=ffp: e.tensor_tensor(out=zt, in0=ffp, in1=fgb_b, op=ALU.add), [bank_t[pb], fgb_t], [zt_t])
        op("act", lambda e: e.activation(out=et, in_=zt, func=AF.Exp, scale=-1.0), [zt_t], [et_t])
        op("act", lambda e: e.activation(out=lt, in_=et, func=AF.Ln, bias=1.0, scale=1.0), [et_t], [lt_t])
        op("pe", lambda e, cps=cps: e.matmul(cps[:, 0:8], lhsT=triN[:, 0:128], rhs=lt, start=True, stop=True), [tri_t, lt_t], [bank_t[pb]])
        op("pe", lambda e, cps=cps: e.matmul(cps[:, 8:16], lhsT=triN[:, 128:256], rhs=lt, start=True, stop=True), [tri_t, lt_t], [bank_t[pb]])
        if i == 0:
            op("dve", lambda e, cps=cps: e.tensor_copy(out=cc[:, 0, :], in_=cps), [bank_t[pb]], [cc_t[0]])
        else:
            op("dve", lambda e, cps=cps, i=i: e.tensor_tensor(out=cc[:, i, :].rearrange("p (a b) -> p a b", a=2),
                                                              in0=cps.rearrange("p (a b) -> p a b", a=2),
                                                              in1=cc[:, i - 1, 8:16].unsqueeze(1).to_broadcast([128, 2, 8]), op=ALU.add),
               [bank_t[pb], cc_t[i - 1]], [cc_t[i]])

    wq_b = A.alloc([128, 8, 128], BF16); wk_b = A.alloc([128, 8, 128], BF16); wv_b = A.alloc([128, 8, 128], BF16)
    wq_t = Trk(); wk_t = Trk(); wv_t = Trk()
    qT = A.alloc([128, SEQ], BF16); kT = A.alloc([128, SEQ], BF16); qT_t = [Trk() for _ in range(8)]; kT_t = [Trk() for _ in range(8)]
    vaug = A.alloc([128, NT, 2, 65], BF16); vaug_t = [Trk() for _ in range(NT)]
    op("pool", lambda e: e.memset(vaug[:, :, :, 64:65], 1.0), [], vaug_t)
    NPT = 6; LA = 3
    pT = [A.alloc([128, 128], BF16) for _ in range(NPT)]; pT_t = [Trk() for _ in range(NPT)]
    NBB = 4
    biasb = [A.alloc([128, NT]) for _ in range(NBB)]; biasb_t = [Trk() for _ in range(NBB)]
    rz = A.alloc([128, 2, 1]); rz_t = Trk(); on = A.alloc([128, 2, 64]); on_t = Trk(); junk64 = A.alloc([128, 64]); junk_t = Trk()
    ssq = A.alloc([128, 2]); ssq_t = Trk(); lnr = A.alloc([128, 2]); lnr_t = Trk(); rs2 = A.alloc([128, 2]); rs2_t = Trk()
    fo = [A.alloc([128, 128], BF16) for _ in range(2)]; fo_t = [Trk(), Trk()]; fo_s = [S.dsem("fo0"), S.dsem("fo1")]
    s_slots = [(banks[k][:, 0:128], bank_t[k]) for k in range(4)]
    o_acc = [(banks[4][:, 0:130].rearrange("p (h d) -> p h d", h=2), bank_t[4]), (banks[5][:, 0:130].rearrange("p (h d) -> p h d", h=2), bank_t[5])]
    epi_n = [0]
    for p in range(4):
        load_w_bf16(wq_b, wq_t, w_in, p * 128, 128, scale=0.125)
        load_w_bf16(wk_b, wk_t, w_in, 512 + p * 128, 128)
        load_w_bf16(wv_b, wv_t, w_in, 1024 + p * 128, 128)
        for tg in range(8):
            for (wb, wt, dst, dt_, pb) in ((wq_b, wq_t, qT, qT_t, 0), (wk_b, wk_t, kT, kT_t, 1)):
                for c in range(8):
                    op("pe", lambda e, wb=wb, c=c, tg=tg, pb=pb: e.matmul(banks[pb][:], lhsT=wb[:, c, :], rhs=xnT[:, c, tg * 512:(tg + 1) * 512],
                                                                          start=(c == 0), stop=(c == 7)),
                       [wt] + xnT_t[tg * 4:tg * 4 + 4], [bank_t[pb]])
                if pb == 0:
                    op("act", lambda e, dst=dst, tg=tg, pb=pb: e.copy(out=dst[:, tg * 512:(tg + 1) * 512], in_=banks[pb][:]), [bank_t[pb]], [dt_[tg]])
                else:
                    op("dve", lambda e, dst=dst, tg=tg, pb=pb: e.tensor_copy(out=dst[:, tg * 512:(tg + 1) * 512], in_=banks[pb][:]), [bank_t[pb]], [dt_[tg]])
        for i in range(NT):
            pb = i % 2
            for c in range(8):
                op("pe", lambda e, c=c, i=i, pb=pb: e.matmul(banks[pb][:, 0:128], lhsT=xnT[:, c, i * 128:(i + 1) * 128], rhs=wv_b[:, c, :],
                                                             start=(c == 0), stop=(c == 7)), [xnT_t[i], wv_t], [bank_t[pb]])
            op("act", lambda e, i=i, pb=pb: e.copy(out=vaug[:, i, :, 0:64], in_=banks[pb][:, 0:128].rearrange("p (h d) -> p h d", h=2)),
               [bank_t[pb]], [vaug_t[i]])
        blocks = [(i, h2, j) for i in range(NT) for h2 in range(2) for j in range(i + 1)]
        nb = len(blocks)
        cur_bias = {}
        for n in range(nb + LA):
            if n % 10 == 0:
                S.replay(pp, 1)
            if n < nb:
                i, h2, j = blocks[n]
                h = 2 * p + h2
                if j == 0:
                    bb_i = (2 * i + h2) % NBB
                    cur_bias[(i, h2)] = bb_i
                    op("dve", lambda e, i=i, h=h, bb_i=bb_i: e.tensor_scalar(out=biasb[bb_i][:, 0:i + 1], in0=cc[:, 0:i + 1, h],
                                                                               scalar1=cc[:, i, 8 + h:9 + h], scalar2=-1.0,
                                                                               op0=ALU.subtract, op1=ALU.mult),
                       cc_t[0:i + 1], [biasb_t[bb_i]])
                bb_i = cur_bias[(i, h2)]
                sl, sl_t = s_slots[n % 4]
                r0 = h2 * 64
                op("pe", lambda e, sl=sl, r0=r0, i=i, j=j: e.matmul(sl, lhsT=kT[r0:r0 + 64, j * 128:(j + 1) * 128], rhs=qT[r0:r0 + 64, i * 128:(i + 1) * 128],
                                                                   start=True, stop=(j != i)), [kT_t[j // 4], qT_t[i // 4]], [sl_t])
                if j == i:
                    op("pe", lambda e, sl=sl: e.matmul(sl, lhsT=ident_b[:], rhs=maskb, start=False, stop=True), [t_const, mask_t], [sl_t])
                pb_ = n % NPT
                op("act", lambda e, sl=sl, pb_=pb_, bb_i=bb_i, j=j: e.activation(out=pT[pb_], in_=sl, func=AF.Exp, bias=biasb[bb_i][:, j:j + 1], scale=1.0),
                   [sl_t, biasb_t[bb_i]], [pT_t[pb_]])
            m = n - LA
            if m >= 0:
                i, h2, j = blocks[m]
                oa, oa_t = o_acc[i % 2]
                pb_ = m % NPT
                op("pe", lambda e, oa=oa, h2=h2, pb_=pb_, i=i, j=j: e.matmul(oa[:, h2, :], lhsT=pT[pb_], rhs=vaug[:, j, h2, :],
                                                                            start=(j == 0), stop=(j == i)), [pT_t[pb_], vaug_t[j]], [oa_t])
                if h2 == 1 and j == i:
                    fb = epi_n[0] % 2; epi_n[0] += 1
                    op("dve", lambda e, oa=oa: e.reciprocal(out=rz, in_=oa[:, :, 64:65]), [oa_t], [rz_t])
                    op("dve", lambda e, oa=oa: e.tensor_tensor(out=on, in0=oa[:, :, 0:64], in1=rz.to_broadcast([128, 2, 64]), op=ALU.mult),
                       [oa_t, rz_t], [on_t])
                    for hh in range(2):
                        op("dve", lambda e, hh=hh: e.scalar_tensor_tensor(out=junk64, in0=on[:, hh, :], scalar=1.0, in1=on[:, hh, :],
                                                                          op0=ALU.mult, op1=ALU.mult, accum_out=ssq[:, hh:hh + 1]),
                           [on_t], [junk_t, ssq_t])
                    op("act", lambda e: e.activation(out=lnr, in_=ssq, func=AF.Ln, bias=RMS_EPS, scale=1.0 / 64.0), [ssq_t], [lnr_t])
                    op("act", lambda e: e.activation(out=rs2, in_=lnr, func=AF.Exp, scale=-0.5), [lnr_t], [rs2_t])
                    op("dve", lambda e: e.tensor_tensor(out=on, in0=on, in1=rs2.unsqueeze(2).to_broadcast([128, 2, 64]), op=ALU.mult),
                       [on_t, rs2_t], [on_t])
                    op("dve", lambda e, fb=fb, p=p: e.tensor_tensor(out=fo[fb].rearrange("p (h d) -> p h d", h=2), in0=on,
                                                                    in1=gfox[:, p * 128:(p + 1) * 128].rearrange("p (h d) -> p h d", h=2), op=ALU.mult),
                       [on_t, gfox_t], [fo_t[fb]])
                    S.dma("sp", lambda e, fb=fb, i=i, p=p: e.dma_start(out=mix_scr[i * 128:(i + 1) * 128, p * 128:(p + 1) * 128], in_=fo[fb]),
                          fo_s[fb], reads=[fo_t[fb]])
    S.barrier()
    if stop == 'B1':
        return finish()

    A.reset(mark_AB)
    hfb = A.alloc([128, 4]); hlb = A.alloc([128, 8]); hp_t = Trk()
    S.dma("sp", lambda e: e.dma_start(out=hfb, in_=hfb_d), s_par, writes=[hp_t, par_chain])
    S.dma("sp", lambda e: e.dma_start(out=hlb, in_=hlb_d), s_par, writes=[hp_t, par_chain])
    nfb = A.alloc([128, 4]); lbv = A.alloc([128, 4]); oml = A.alloc([128, 4]); tl = A.alloc([128, 4]); hq_t = Trk()
    op("dve", lambda e: e.tensor_scalar(out=nfb, in0=hfb, scalar1=-1.0, scalar2=None, op0=ALU.mult), [hp_t], [hq_t])
    op("dve", lambda e: e.tensor_tensor(out=tl, in0=hlb[:, 0:4], in1=hlb[:, 4:8], op=ALU.subtract), [hp_t], [hq_t])
    op("act", lambda e: e.activation(out=tl, in_=tl, func=AF.Exp, scale=-1.0), [hq_t], [hq_t])
    op("dve", lambda e: e.tensor_scalar(out=tl, in0=tl, scalar1=1.0, scalar2=None, op0=ALU.add), [hq_t], [hq_t])
    op("dve", lambda e: e.reciprocal(out=lbv, in_=tl), [hq_t], [hq_t])
    op("dve", lambda e: e.tensor_scalar(out=oml, in0=lbv, scalar1=-1.0, scalar2=1.0, op0=ALU.mult, op1=ALU.add), [hq_t], [hq_t])
    ghg = A.alloc([128, 512]); ghg_t = Trk()
    bcast_load(ghg, ghg_t, hgrn_og, s_par)
    cmask = A.alloc([128, 512]); mA = A.alloc([128, 512]); mB = A.alloc([128, 512]); cm_t = Trk()
    op("pool", lambda e: e.memset(cmask, 1.0), [], [cm_t])
    op("pool", lambda e: e.memset(cmask.rearrange("p (c t) -> p c t", t=64)[:, :, 0:1], 0.0), [cm_t], [cm_t])
    op("pool", lambda e: e.memset(mA, 0.0), [], [cm_t])
    op("pool", lambda e: e.memset(mA.rearrange("p (c t) -> p c t", t=128)[:, :, 0:64], 1.0), [cm_t], [cm_t])
    op("pool", lambda e: e.memset(mB, 0.0), [], [cm_t])
    op("pool", lambda e: e.memset(mB.rearrange("p (c t) -> p c t", t=128)[:, :, 64:128], 1.0), [cm_t], [cm_t])
    bdm = A.alloc([128, 128]); bdm_t = Trk()
    op("pool", lambda e: e.memset(bdm, 1.0), [], [bdm_t])
    op("pool", lambda e: e.affine_select(out=bdm, in_=bdm, pattern=[[1, 128]], compare_op=ALU.is_ge, fill=0.0, base=0, channel_multiplier=-1),
       [bdm_t], [bdm_t])
    op("pool", lambda e: e.memset(bdm[0:64, 64:128], 0.0), [bdm_t], [bdm_t])
    whq = A.alloc([128, 8, 128], BF16); whf = A.alloc([128, 8, 128], BF16); whi = A.alloc([128, 8, 128], BF16); whg = A.alloc([128, 8, 128], BF16)
    whq_t = Trk(); whf_t = Trk(); whi_t = Trk(); whg_t = Trk()
    v_tok = [A.alloc([128, 4, 128], BF16) for _ in range(2)]; v_t = [[Trk() for _ in range(4)] for _ in range(2)]
    gg = [A.alloc([128, 4, 128]) for _ in range(2)]; gg_t = [[Trk() for _ in range(4)] for _ in range(2)]
    t_e = A.alloc([128, 512]); t_f = A.alloc([128, 512]); t_g = A.alloc([128, 512]); t_k = A.alloc([128, 512]); t_b = A.alloc([128, 512])
    t_x = A.alloc([128, 512]); t_e2 = A.alloc([128, 512]); t_d = A.alloc([128, 512])
    te_t, tf_t, tg_t, tk_t, tb_t, tx_t, te2_t, td_t = (Trk() for _ in range(8))
    qtf = [A.alloc([128, 512], BF16) for _ in range(2)]; qtl = [A.alloc([128, 512], BF16) for _ in range(2)]; qth = [A.alloc([128, 512], BF16) for _ in range(2)]
    ktT = [A.alloc([128, 512], BF16) for _ in range(2)]; kdT = [A.alloc([128, 512], BF16) for _ in range(2)]
    kd_tok = [A.alloc([128, 4, 128], BF16) for _ in range(2)]
    ebl = [A.alloc([128, 8]) for _ in range(2)]
    qtf_t = [Trk() for _ in range(2)]; qtl_t = [Trk() for _ in range(2)]; qth_t = [Trk() for _ in range(2)]; ktT_t = [Trk() for _ in range(2)]
    kdT_t = [Trk() for _ in range(2)]; kdk_t = [Trk() for _ in range(2)]; ebl_t = [Trk() for _ in range(2)]
    Sf = A.alloc([128, 128]); Sf_t = Trk()
    Sb = [A.alloc([128, 128], BF16) for _ in range(2)]; Sb_t = [Trk(), Trk()]
    sm = [A.alloc([128, 128], BF16) for _ in range(2)]; sm_t = [Trk(), Trk()]
    o_sb = [A.alloc([128, 128]) for _ in range(2)]; osb_t = [Trk(), Trk()]
    junk128 = A.alloc([128, 128]); j128_t = Trk()
    hss = [A.alloc([128, 1]) for _ in range(2)]; hln = [A.alloc([128, 1]) for _ in range(2)]; hrs = [A.alloc([128, 1]) for _ in range(2)]
    hss_t = [Trk(), Trk()]; hln_t = [Trk(), Trk()]; hrs_t = [Trk(), Trk()]
    yb = [A.alloc([128, 128], BF16) for _ in range(2)]; yb_t = [Trk(), Trk()]; yb_s = [S.dsem("yb0"), S.dsem("yb1")]
    silu_t = A.alloc([128, 128]); silu_tt = Trk()
    sc_sl = [banks[4][:, 0:128]] * 2; sc_t = [bank_t[4]] * 2
    o_sl = [banks[5][:, 0:128]] * 2; o_t = [bank_t[5]] * 2
    ds_sl = [banks[6][:, k * 128:(k + 1) * 128] for k in range(2)]; ds_t = [bank_t[6]] * 2
    pv_sl = [banks[2][:, 0:128], banks[3][:, 0:128]]; pv_t = [bank_t[2], bank_t[3]]
    epi2 = [0]
    for h in range(4):
        load_w_bf16(whq, whq_t, w_in, 1544 + h * 128, 128); load_w_bf16(whf, whf_t, w_in, 2056 + h * 128, 128)
        load_w_bf16(whi, whi_t, w_in, 2568 + h * 128, 128); load_w_bf16(whg, whg_t, w_in, 3080 + h * 128, 128)
        op("dve", lambda e: e.memset(Sf, 0.0), [], [Sf_t])
        op("pool", lambda e: e.memset(Sb[0], 0.0), [], [Sb_t[0]])
        hc = slice(h * 128, (h + 1) * 128)
        def front(tg, h=h, hc=hc):
            d = tg % 2
            xs = xnT_t[tg * 4:tg * 4 + 4]
            for tt in range(4):
                tok = (tg * 4 + tt) * 128
                for c in range(8):
                    op("pe", lambda e, c=c, tok=tok: e.matmul(pv_sl[0], lhsT=xnT[:, c, tok:tok + 128], rhs=whi[:, c, :], start=(c == 0), stop=(c == 7)),
                       [xnT_t[tg * 4 + tt], whi_t], [pv_t[0]])
                op("act", lambda e, tt=tt, d=d: e.copy(out=v_tok[d][:, tt, :], in_=pv_sl[0]), [pv_t[0]], [v_t[d][tt]])
                for c in range(8):
                    op("pe", lambda e, c=c, tok=tok: e.matmul(pv_sl[1], lhsT=xnT[:, c, tok:tok + 128], rhs=whg[:, c, :], start=(c == 0), stop=(c == 7)),
                       [xnT_t[tg * 4 + tt], whg_t], [pv_t[1]])
                op("act", lambda e: e.activation(out=silu_t, in_=pv_sl[1], func=AF.Silu), [pv_t[1]], [silu_tt])
                op("dve", lambda e, tt=tt, d=d, hc=hc: e.tensor_tensor(out=gg[d][:, tt, :], in0=silu_t, in1=ghg[:, hc], op=ALU.mult), [silu_tt, ghg_t], [gg_t[d][tt]])
            for (wb, wt, pb) in ((whq, whq_t, 0), (whf, whf_t, 1)):
                for c in range(8):
                    op("pe", lambda e, wb=wb, c=c, pb=pb, tg=tg: e.matmul(banks[pb][:], lhsT=wb[:, c, :],
                                                                          rhs=xnT[:, c, tg * 512:(tg + 1) * 512], start=(c == 0), stop=(c == 7)),
                       [wt] + xs, [bank_t[pb]])
            op("act", lambda e, h=h: e.activation(out=t_f, in_=banks[1][:], func=AF.Sigmoid, bias=hfb[:, h:h + 1], scale=1.0), [bank_t[1], hp_t], [tf_t])
            op("dve", lambda e, h=h: e.tensor_scalar(out=t_f, in0=t_f, scalar1=oml[:, h:h + 1], scalar2=lbv[:, h:h + 1], op0=ALU.mult, op1=ALU.add),
               [tf_t, hq_t], [tf_t])
            op("act", lambda e: e.activation(out=t_g, in_=t_f, func=AF.Ln), [tf_t], [tg_t])
            op("dve", lambda e: e.tensor_scalar(out=t_k, in0=t_f, scalar1=-1.0, scalar2=1.0, op0=ALU.mult, op1=ALU.add), [tf_t], [tk_t])
            op("dve", lambda e: e.tensor_tensor_scan(out=t_b, data0=cmask, data1=t_g, initial=0.0, op0=ALU.mult, op1=ALU.add), [cm_t, tg_t], [tb_t])
            op("act", lambda e: e.activation(out=t_e, in_=t_b, func=AF.Exp), [tb_t], [te_t])
            op("dve", lambda e: e.tensor_tensor(out=t_x, in0=banks[0][:], in1=t_e, op=ALU.mult), [bank_t[0], te_t], [tx_t])
            op("act", lambda e, d=d: e.copy(out=qtf[d], in_=t_x), [tx_t], [qtf_t[d]])
            op("dve", lambda e, d=d: e.tensor_tensor(out=qtl[d], in0=t_x, in1=mA, op=ALU.mult), [tx_t, cm_t], [qtl_t[d]])
            op("dve", lambda e, d=d: e.tensor_tensor(out=qth[d], in0=t_x, in1=mB, op=ALU.mult), [tx_t, cm_t], [qth_t[d]])
            op("act", lambda e: e.activation(out=t_e2, in_=t_b, func=AF.Exp, scale=-1.0), [tb_t], [te2_t])
            op("dve", lambda e, d=d: e.tensor_tensor(out=ktT[d], in0=t_k, in1=t_e2, op=ALU.mult), [tk_t, te2_t], [ktT_t[d]])
            tb3 = t_b.rearrange("p (c t) -> p c t", t=64)
            op("dve", lambda e, tb3=tb3: e.tensor_tensor(out=t_d.rearrange("p (c t) -> p c t", t=64), in0=tb3[:, :, 63:64].to_broadcast([128, 8, 64]),
                                                          in1=tb3, op=ALU.subtract), [tb_t], [td_t])
            op("act", lambda e: e.activation(out=t_d, in_=t_d, func=AF.Exp), [td_t], [td_t])
            op("dve", lambda e, d=d: e.tensor_tensor(out=kdT[d], in0=t_k, in1=t_d, op=ALU.mult), [tk_t, td_t], [kdT_t[d]])
            op("act", lambda e, d=d, tb3=tb3: e.activation(out=ebl[d].unsqueeze(2), in_=tb3[:, :, 63:64], func=AF.Exp), [tb_t], [ebl_t[d]])
            pv = tb_v[d]
            for tt in range(4):
                op("pe", lambda e, pv=pv, tt=tt, d=d: e.transpose(out=pv[:, tt, :], in_=kdT[d][:, tt * 128:(tt + 1) * 128], identity=ident_b[:]),
                   [kdT_t[d], t_const], [tbk_t[d]])
            op("dve", lambda e, pv=pv, d=d: e.tensor_copy(out=kd_tok[d], in_=pv), [tbk_t[d]], [kdk_t[d]])
        def chunks(tg, deferred, h=h, hc=hc):
            d = tg % 2
            def opr(*a):
                op(*a)
                S.iter += 1
                S.replay(deferred, 2, lag=1)
            for tt in range(4):
                S.replay(pp, 3)
                tile_i = tg * 4 + tt
                cs = slice(tt * 128, (tt + 1) * 128)
                k2 = tt % 2
                opr("pe", lambda e, d=d, cs=cs, k2=k2: e.matmul(sc_sl[k2], lhsT=ktT[d][:, cs], rhs=qtf[d][:, cs], start=True, stop=True),
                   [ktT_t[d], qtf_t[d]], [sc_t[k2]])
                opr("dve", lambda e, k2=k2: e.tensor_tensor(out=sm[k2], in0=sc_sl[k2], in1=bdm, op=ALU.mult), [sc_t[k2], bdm_t], [sm_t[k2]])
                opr("pe", lambda e, k2=k2, tt=tt, d=d: e.matmul(o_sl[k2], lhsT=sm[k2], rhs=v_tok[d][:, tt, :], start=True, stop=False),
                   [sm_t[k2], v_t[d][tt]], [o_t[k2]])
                opr("pe", lambda e, k2=k2, cs=cs, d=d: e.matmul(o_sl[k2], lhsT=qtl[d][:, cs], rhs=Sb[0], start=False, stop=False),
                   [qtl_t[d], Sb_t[0]], [o_t[k2]])
                opr("pe", lambda e, tt=tt, d=d: e.matmul(ds_sl[0], lhsT=kd_tok[d][0:64, tt, :], rhs=v_tok[d][0:64, tt, :], start=True, stop=True),
                   [kdk_t[d], v_t[d][tt]], [ds_t[0]])
                opr("dve", lambda e, tt=tt, d=d: e.scalar_tensor_tensor(out=Sf, in0=Sf, scalar=ebl[d][:, 2 * tt:2 * tt + 1], in1=ds_sl[0],
                                                                       op0=ALU.mult, op1=ALU.add), [Sf_t, ebl_t[d], ds_t[0]], [Sf_t])
                opr("act", lambda e: e.copy(out=Sb[1], in_=Sf), [Sf_t], [Sb_t[1]])
                opr("pe", lambda e, k2=k2, cs=cs, d=d: e.matmul(o_sl[k2], lhsT=qth[d][:, cs], rhs=Sb[1], start=False, stop=True),
                   [qth_t[d], Sb_t[1]], [o_t[k2]])
                opr("pe", lambda e, tt=tt, d=d: e.matmul(ds_sl[1], lhsT=kd_tok[d][64:128, tt, :], rhs=v_tok[d][64:128, tt, :], start=True, stop=True),
                   [kdk_t[d], v_t[d][tt]], [ds_t[1]])
                opr("dve", lambda e, tt=tt, d=d: e.scalar_tensor_tensor(out=Sf, in0=Sf, scalar=ebl[d][:, 2 * tt + 1:2 * tt + 2], in1=ds_sl[1],
                                                                       op0=ALU.mult, op1=ALU.add), [Sf_t, ebl_t[d], ds_t[1]], [Sf_t])
                opr("act", lambda e: e.copy(out=Sb[0], in_=Sf), [Sf_t], [Sb_t[0]])
                eb = epi2[0] % 2; epi2[0] += 1
                opr("act", lambda e, k2=k2, eb=eb: e.copy(out=o_sb[eb], in_=o_sl[k2]), [o_t[k2]], [osb_t[eb]])
                opr("dve", lambda e, eb=eb: e.scalar_tensor_tensor(out=junk128, in0=o_sb[eb], scalar=1.0, in1=o_sb[eb], op0=ALU.mult, op1=ALU.mult,
                                                                  accum_out=hss[eb]), [osb_t[eb]], [j128_t, hss_t[eb]])
                opr("act", lambda e, eb=eb: e.activation(out=hln[eb], in_=hss[eb], func=AF.Ln, bias=RMS_EPS, scale=1.0 / 128.0), [hss_t[eb]], [hln_t[eb]])
                opr("act", lambda e, eb=eb: e.activation(out=hrs[eb], in_=hln[eb], func=AF.Exp, scale=-0.5), [hln_t[eb]], [hrs_t[eb]])
                opr("dve", lambda e, eb=eb, tt=tt, d=d: e.scalar_tensor_tensor(out=yb[eb], in0=o_sb[eb], scalar=hrs[eb][:, 0:1], in1=gg[d][:, tt, :],
                                                                              op0=ALU.mult, op1=ALU.mult), [osb_t[eb], hrs_t[eb], gg_t[d][tt]], [yb_t[eb]])
                S.dma("sp", lambda e, eb=eb, tile_i=tile_i, h=h: e.dma_start(out=mix_scr[tile_i * 128:(tile_i + 1) * 128, 512 + h * 128:512 + (h + 1) * 128],
                                                                            in_=yb[eb]), yb_s[eb], reads=[yb_t[eb]])
        front(0)
        for tg in range(8):
            deferred = S.capture(lambda: front(tg + 1)) if tg + 1 < 8 else []
            chunks(tg, deferred)
            S.replay(deferred, len(deferred))
    S.replay(pp, len(pp))
    S.barrier()
    if stop == 'B2':
        return finish()

    A.reset()
    wqp = A.alloc([128, 8, 2048], BF16); wqp_t = Trk()
    wo = A.alloc([128, 8, 1024], BF16); wo_t = Trk()
    kTb = A.alloc([128, 16, 128], BF16); kTb_t = Trk()
    load_w_bf16(wo, wo_t, w_out, 0, 1024)
    load_w_bf16(wqp, wqp_t, w_q, 0, 2048)
    for k in range(2):
        b = wst_n[0] % 2; wst_n[0] += 1
        S.dma("sp", lambda e, b=b, k=k: e.dma_start(out=wst[b], in_=keysT_d[:, k * 8:(k + 1) * 8, :]), wst_s[b], writes=[wst_t[b]])
        op("act", lambda e, b=b, k=k: e.copy(out=kTb[:, k * 8:(k + 1) * 8, :], in_=wst[b]), [wst_t[b]], [kTb_t])
    g2b = A.alloc([128, DM]); b2b = A.alloc([128, DM]); g3b = A.alloc([128, DM]); b3b = A.alloc([128, DM]); gb2_t = Trk(); gb3_t = Trk()
    bcast_load(g2b, gb2_t, ln_mix_g, s_par); bcast_load(b2b, gb2_t, ln_mix_b, s_par)
    bcast_load(g3b, gb3_t, ln_ffn_g, s_par); bcast_load(b3b, gb3_t, ln_ffn_b, s_par)
    iota16 = A.alloc([128, 16]); io_t = Trk()
    op("pool", lambda e: e.iota(iota16, pattern=[[1, 16]], base=0, channel_multiplier=0, allow_small_or_imprecise_dtypes=True), [], [io_t])
    NG = 12
    gbuf = [A.alloc([128, 2 * DM], BF16) for _ in range(NG)]; gbuf_t = [Trk() for _ in range(NG)]; gbuf_s = [S.dsem("g%d" % k) for k in range(NG)]
    mt = A.alloc([128, DM], BF16); mt_t = Trk(); mt_s = S.dsem("mt")
    mT = A.alloc([128, 8, 128], BF16); mT_t = Trk()
    xnt = [A.alloc([128, DM]) for _ in range(2)]; xnt_t = [Trk(), Trk()]; xnt_s = [S.dsem("xnt0"), S.dsem("xnt1")]
    xn2 = [A.alloc([128, DM]) for _ in range(2)]; xn2_t = [Trk(), Trk()]
    idx = [A.alloc([128, 128], I32) for _ in range(2)]; idx_t = [Trk(), Trk()]
    gates = [A.alloc([128, 128]) for _ in range(2)]; gate_t = [Trk(), Trk()]
    ot_s = [S.dsem("ot0"), S.dsem("ot1")]
    xn2b = [A.alloc([128, DM], BF16) for _ in range(2)]; xn2b_t = [Trk(), Trk()]
    x2T = A.alloc([128, 8, 128], BF16); x2T_t = Trk()
    qTs = A.alloc([128, 16, 128], BF16); qTs_t = Trk()
    s_sb = A.alloc([128, 2048]); ssb_t = Trk()
    sR = A.alloc([128, 128]); sR_t = Trk()
    v16 = A.alloc([128, 8, 2, 16]); v16_t = Trk(); i16 = A.alloc([128, 8, 2, 16], U32); i16_t = Trk()
    cand = A.alloc([128, 8, 256]); cand_t = Trk(); cR = A.alloc([128, 256]); cR_t = Trk()
    b16 = A.alloc([128, 8, 16]); b16_t = Trk(); pos = A.alloc([128, 8, 16], U32); pos_t = Trk()
    pa = A.alloc([128, 8, 16], U32); pbq = A.alloc([128, 8, 16], U32); paf = A.alloc([128, 8, 16]); pbf = A.alloc([128, 8, 16]); dec_t = Trk()
    i16f = A.alloc([128, 8, 2, 16]); i16f_t = Trk()
    eq = cand.rearrange("p h (a b) -> p h a b", a=16); eq_t = cand_t
    If = A.alloc([128, 8, 16]); Jf = A.alloc([128, 8, 16]); IJ_t = Trk()
    gd = A.alloc([128, 8, 16]); gsum = A.alloc([128, 8]); gd_t = Trk()
    GS = 1
    xu = A.alloc([128, 128]); xu_t = [Trk() for _ in range(128 // GS)]; hg = A.alloc([128, 128]); hg_t = [Trk() for _ in range(128 // GS)]
    hf = A.alloc([128, 128]); hf_t = [Trk() for _ in range(128 // GS)]
    NPR = 4
    prod = [A.alloc([128, DM], BF16) for _ in range(NPR)]; prod_t = [Trk() for _ in range(NPR)]; prc = [0]
    NDG = 3
    dg = [A.alloc([128, 2, 128], BF16) for _ in range(NDG)]; dg_t = [Trk() for _ in range(NDG)]; dgc = [0]
    lnC = ln_tmp(A)
    lnD = lnC
    gcount = [0]

    def pre(i):
        q = i % 2
        rows = slice(i * 128, (i + 1) * 128)
        S.dma("sp", lambda e: e.dma_start(out=mt, in_=mix_scr[rows, :]), mt_s, writes=[mt_t])
        S.dma("sp", lambda e: e.dma_start(out=xnt[q], in_=xn_scr[rows, :]), xnt_s[q], writes=[xnt_t[q]])
        for half in range(2):
            pv = tb_v[half]
            for c4 in range(4):
                c = half * 4 + c4
                op("pe", lambda e, pv=pv, c4=c4, c=c: e.transpose(out=pv[:, c4, :], in_=mt[:, c * 128:(c + 1) * 128], identity=ident_b[:]),
                   [mt_t, t_const], [tbk_t[half]])
            op("act", lambda e, pv=pv, half=half: e.copy(out=mT[:, half * 4:half * 4 + 4, :], in_=pv), [tbk_t[half]], [mT_t])
        for nh in range(2):
            for c in range(8):
                op("pe", lambda e, nh=nh, c=c: e.matmul(banks[nh][:], lhsT=mT[:, c, :], rhs=wo[:, c, nh * 512:(nh + 1) * 512], start=(c == 0), stop=(c == 7)),
                   [mT_t, wo_t], [bank_t[nh]])
            op("dve", lambda e, nh=nh: e.scalar_tensor_tensor(out=xnt[q][:, nh * 512:(nh + 1) * 512], in0=xnt[q][:, nh * 512:(nh + 1) * 512], scalar=ALPHA,
                                                              in1=banks[nh][:], op0=ALU.mult, op1=ALU.add), [xnt_t[q], bank_t[nh]], [xnt_t[q]])
        layer_norm(xnt[q], [xnt_t[q]], g2b, b2b, gb2_t, xn2[q], xn2_t[q], xn2b[q], xn2b_t[q], lnC, gb_eng="dve")
        for half in range(2):
            pv = tb_v[half]
            for c4 in range(4):
                c = half * 4 + c4
                op("pe", lambda e, pv=pv, c4=c4, c=c: e.transpose(out=pv[:, c4, :], in_=xn2b[q][:, c * 128:(c + 1) * 128], identity=ident_b[:]),
                   [xn2b_t[q], t_const], [tbk_t[half]])
            op("act", lambda e, pv=pv, half=half: e.copy(out=x2T[:, half * 4:half * 4 + 4, :], in_=pv), [tbk_t[half]], [x2T_t])
        for c4g in range(4):
            for cq in range(4):
                cidx = c4g * 4 + cq
                for k in range(8):
                    op("pe", lambda e, cq=cq, cidx=cidx, k=k: e.matmul(banks[6][:, cq * 128:(cq + 1) * 128], lhsT=wqp[:, k, cidx * 128:(cidx + 1) * 128],
                                                                      rhs=x2T[:, k, :], start=(k == 0), stop=(k == 7)), [wqp_t, x2T_t], [bank_t[6]])
            op("act", lambda e, c4g=c4g: e.copy(out=qTs[:, c4g * 4:c4g * 4 + 4, :], in_=banks[6][:].rearrange("p (c t) -> p c t", c=4)),
               [bank_t[6]], [qTs_t])
        for half2 in range(2):
            for c8 in range(8):
                cidx = half2 * 8 + c8
                bk = 2 + c8 // 4
                op("pe", lambda e, cidx=cidx, bk=bk: e.matmul(banks[bk][:, (cidx % 4) * 128:(cidx % 4 + 1) * 128], lhsT=qTs[:, cidx, :], rhs=kTb[:, cidx, :],
                                                              start=True, stop=True), [qTs_t, kTb_t], [bank_t[bk]])
            for bq in range(2):
                op("act", lambda e, bq=bq, half2=half2: e.copy(out=s_sb[:, (half2 * 2 + bq) * 512:(half2 * 2 + bq + 1) * 512], in_=banks[2 + bq][:]),
                   [bank_t[2 + bq]], [ssb_t])
        for h in range(8):
            for pp in range(2):
                sl = s_sb[:, (2 * h + pp) * 128:(2 * h + pp + 1) * 128]
                op("dve", lambda e, sl=sl, h=h, pp=pp: e.max(out=v16[:, h, pp, 0:8], in_=sl), [ssb_t], [v16_t])
                op("dve", lambda e, sl=sl, h=h, pp=pp: e.max_index(out=i16[:, h, pp, 0:8], in_max=v16[:, h, pp, 0:8], in_values=sl), [ssb_t, v16_t], [i16_t])
                op("dve", lambda e, sl=sl, h=h, pp=pp: e.match_replace(out=sR, in_to_replace=v16[:, h, pp, 0:8], in_values=sl, imm_value=NEG),
                   [ssb_t, v16_t], [sR_t])
                op("dve", lambda e, h=h, pp=pp: e.max(out=v16[:, h, pp, 8:16], in_=sR), [sR_t], [v16_t])
                op("dve", lambda e, h=h, pp=pp: e.max_index(out=i16[:, h, pp, 8:16], in_max=v16[:, h, pp, 8:16], in_values=sR), [sR_t, v16_t], [i16_t])
            ch = cand[:, h, :]
            op("dve", lambda e, h=h, ch=ch: e.tensor_tensor(out=ch.rearrange("p (a b) -> p a b", a=16), in0=v16[:, h, 0, :].unsqueeze(2).to_broadcast([128, 16, 16]),
                                                            in1=v16[:, h, 1, :].unsqueeze(1).to_broadcast([128, 16, 16]), op=ALU.add), [v16_t], [cand_t])
            op("dve", lambda e, h=h, ch=ch: e.max(out=b16[:, h, 0:8], in_=ch), [cand_t], [b16_t])
            op("dve", lambda e, h=h, ch=ch: e.max_index(out=pos[:, h, 0:8], in_max=b16[:, h, 0:8], in_values=ch), [cand_t, b16_t], [pos_t])
            op("dve", lambda e, h=h, ch=ch: e.match_replace(out=cR, in_to_replace=b16[:, h, 0:8], in_values=ch, imm_value=NEG), [cand_t, b16_t], [cR_t])
            op("dve", lambda e, h=h: e.max(out=b16[:, h, 8:16], in_=cR), [cR_t], [b16_t])
            op("dve", lambda e, h=h: e.max_index(out=pos[:, h, 8:16], in_max=b16[:, h, 8:16], in_values=cR), [cR_t, b16_t], [pos_t])
        op("dve", lambda e: e.tensor_single_scalar(out=pa, in_=pos, scalar=4, op=ALU.logical_shift_right), [pos_t], [dec_t])
        op("dve", lambda e: e.tensor_single_scalar(out=pbq, in_=pos, scalar=15, op=ALU.bitwise_and), [pos_t], [dec_t])
        op("dve", lambda e: e.tensor_copy(out=paf, in_=pa), [dec_t], [dec_t])
        op("dve", lambda e: e.tensor_copy(out=pbf, in_=pbq), [dec_t], [dec_t])
        op("dve", lambda e: e.tensor_copy(out=i16f, in_=i16), [i16_t], [i16f_t])
        io4 = iota16.unsqueeze(1).unsqueeze(1).to_broadcast([128, 4, 16, 16])
        for (src, half, dst) in ((paf, 0, If), (pbf, 1, Jf)):
            for hh_ in range(2):
                hs = slice(hh_ * 4, hh_ * 4 + 4)
                op("dve", lambda e, src=src, hs=hs: e.tensor_tensor(out=eq[:, hs], in0=src[:, hs].unsqueeze(3).to_broadcast([128, 4, 16, 16]), in1=io4, op=ALU.is_equal),
                   [dec_t, io_t], [eq_t])
                op("dve", lambda e, half=half, hs=hs: e.tensor_tensor(out=eq[:, hs], in0=eq[:, hs], in1=i16f[:, hs, half, :].unsqueeze(2).to_broadcast([128, 4, 16, 16]), op=ALU.mult),
                   [eq_t, i16f_t], [eq_t])
                op("dve", lambda e, dst=dst, hs=hs: e.tensor_reduce(out=dst[:, hs], in_=eq[:, hs], axis=AX.X, op=ALU.add), [eq_t], [IJ_t])
        op("dve", lambda e: e.scalar_tensor_tensor(out=idx[q].rearrange("p (h k) -> p h k", h=8), in0=If, scalar=128.0, in1=Jf, op0=ALU.mult, op1=ALU.add),
           [IJ_t], [idx_t[q]])
        op("dve", lambda e: e.tensor_tensor(out=gd, in0=b16, in1=b16[:, :, 0:1].to_broadcast([128, 8, 16]), op=ALU.subtract), [b16_t], [gd_t])
        op("act", lambda e: e.activation(out=gd, in_=gd, func=AF.Exp), [gd_t], [gd_t])
        op("dve", lambda e: e.tensor_reduce(out=gsum, in_=gd, axis=AX.X, op=ALU.add), [gd_t], [gd_t])
        op("dve", lambda e: e.reciprocal(out=gsum, in_=gsum), [gd_t], [gd_t])
        op("dve", lambda e: e.tensor_tensor(out=gates[q].rearrange("p (h k) -> p h k", h=8), in0=gd, in1=gsum.unsqueeze(2).to_broadcast([128, 8, 16]), op=ALU.mult),
           [gd_t], [gate_t[q]])

    def gather(i, m):
        q = i % 2
        g = gcount[0] % NG; gcount[0] += 1
        S.dma("pool", lambda e: e.indirect_dma_start(out=gbuf[g], out_offset=None, in_=uv_scr[:, :],
                                                      in_offset=bass.IndirectOffsetOnAxis(ap=idx[q][:, m:m + 1], axis=0)),
              gbuf_s[g], reads=[idx_t[q]], writes=[gbuf_t[g]])
        return g

    def post_a(i):
        q = i % 2
        for nh in range(2):
            op("dve", lambda e, nh=nh: e.scalar_tensor_tensor(out=xnt[q][:, nh * 512:(nh + 1) * 512], in0=xn2[q][:, nh * 512:(nh + 1) * 512], scalar=ALPHA,
                                                              in1=banks[4 + nh][:], op0=ALU.mult, op1=ALU.add), [xn2_t[q], bank_t[4 + nh]], [xnt_t[q]])
    def post_b(i):
        q = i % 2
        rows = slice(i * 128, (i + 1) * 128)
        layer_norm(xnt[q], [xnt_t[q]], g3b, b3b, gb3_t, xnt[q], xnt_t[q], None, None, lnD, gb_eng="dve")
        S.dma("sp", lambda e: e.dma_start(out=out_d[rows, :], in_=xnt[q]), ot_s[q], reads=[xnt_t[q]])

    if os.environ.get("KDBG"):
        print("arena use phase C:", {str(k): v for k, v in A.off.items()})
    pre(0)
    for i in range(NT):
        q = i % 2
        def _defer():
            if i >= 1: post_b(i - 1)
            if i + 1 < NT: pre(i + 1)
        deferred = S.capture(_defer)
        per = max(1, -(-len(deferred) // 115))
        gof = {}
        def st1(m, q=q, i=i):
            g = gather(i, m); gof[m] = g
            pk = prc[0] % NPR; prc[0] += 1
            op("dve", lambda e: e.tensor_tensor(out=prod[pk], in0=gbuf[g][:, 0:DM], in1=xn2b[q], op=ALU.mult), [gbuf_t[g], xn2b_t[q]], [prod_t[pk]])
            op("act", lambda e: e.activation(out=prod[pk], in_=prod[pk], func=AF.Copy, accum_out=xu[:, m:m + 1]), [prod_t[pk]], [prod_t[pk], xu_t[m // 2]])
        def st2a(p):
            cs = slice(2 * p, 2 * p + 2)
            op("act", lambda e: e.activation(out=hg[:, cs], in_=xu[:, cs], func=AF.Gelu), [xu_t[p]], [hg_t[p]])
        def st2b(p, q=q):
            cs = slice(2 * p, 2 * p + 2)
            op("dve", lambda e: e.tensor_tensor(out=hf[:, cs], in0=hg[:, cs], in1=gates[q][:, cs], op=ALU.mult), [hg_t[p], gate_t[q]], [hf_t[p]])
        def st3(p):
            d_ = dgc[0] % NDG; dgc[0] += 1
            op("dve", lambda e: e.tensor_tensor(out=dg[d_], in0=ident_b[:].unsqueeze(1).to_broadcast([128, 2, 128]),
                                                in1=hf[:, 2 * p:2 * p + 2].unsqueeze(2).to_broadcast([128, 2, 128]), op=ALU.mult), [hf_t[p], t_const], [dg_t[d_]])
            for k_ in range(2):
                m = 2 * p + k_
                g = gof[m]
                for nh in range(2):
                    op("pe", lambda e, nh=nh, k_=k_, g=g, m=m: e.matmul(banks[4 + nh][:], lhsT=dg[d_][:, k_, :], rhs=gbuf[g][:, DM + nh * 512:DM + (nh + 1) * 512],
                                                                       start=(m == 0), stop=(m == 127)), [dg_t[d_], gbuf_t[g]], [bank_t[4 + nh]])
        for m in range(128 + 5):
            if m >= 4 and (m - 4) % 2 == 0 and (m - 4) // 2 < 64: st3((m - 4) // 2)
            if m >= 3 and (m - 3) % 2 == 0 and (m - 3) // 2 < 64: st2b((m - 3) // 2)
            if m >= 2 and (m - 2) % 2 == 0 and (m - 2) // 2 < 64: st2a((m - 2) // 2)
            if m < 128: st1(m)
            S.iter += 1
            S.replay(deferred, per + 1, lag=2)
        S.replay(deferred, len(deferred))
        post_a(i)
    post_b(NT - 1)
    S.barrier(["sp"])
    S.emit()
    es.close()
    return nc


_CACHE = {}


def kernel(**inputs):
    x = np.ascontiguousarray(np.asarray(inputs["x"], dtype=np.float32))
    f = lambda k: np.ascontiguousarray(np.asarray(inputs[k], dtype=np.float32))
    shared = {
        "ln_in_g": f("ln_in_g"), "ln_in_b": f("ln_in_b"),
        "w_in": f("w_in")[0],
        "fox_fgate_b": f("fox_fgate_b")[0],
        "hgrn_fgate_b_fm": np.ascontiguousarray(f("hgrn_fgate_b")[0].reshape(4, 128).T),
        "hgrn_lb_fm": np.ascontiguousarray(f("hgrn_lb_logits").reshape(2, 4, 128).transpose(2, 0, 1).reshape(128, 8)),
        "fox_out_g": f("fox_out_g")[0], "hgrn_out_g": f("hgrn_out_g")[0],
        "w_out": f("w_out")[0],
        "ln_mix_g": f("ln_mix_g")[0], "ln_mix_b": f("ln_mix_b")[0],
        "peer_w_q": f("peer_w_q")[0],
        "keysT": np.ascontiguousarray(f("peer_sub_keys")[0].reshape(16, 128, 128).transpose(2, 0, 1)),
        "peer_u": f("peer_u")[0], "peer_v": f("peer_v")[0],
        "ln_ffn_g": f("ln_ffn_g")[0], "ln_ffn_b": f("ln_ffn_b")[0],
    }
    if "nc" not in _CACHE:
        _CACHE["nc"] = build_program()
    nc = _CACHE["nc"]
    in_maps = []
    for c in range(8):
        m = dict(shared); m["x"] = x[c]
        in_maps.append(m)
    res = run_bass_kernel_spmd(nc, in_maps, core_ids=list(range(8)))
    out = np.stack([np.asarray(r["out"], dtype=np.float32).reshape(SEQ, DM) for r in res.results], axis=0)
    return out
```
